# Optimizing a Trainium2 kernel written in Bass

```python
import math
import jax
import jax.numpy as jnp
from jax import lax
import numpy as np

D_MODEL = 1024
BATCH = 2
SEQ = 8192
DEPTH = 2

GRID_W = 64
CTX_LEN = 256
EPS = 1e-6
MLP_HIDDEN = 4 * D_MODEL
N_MOD = 6
N_EVEN = (DEPTH + 1) // 2
N_ODD = DEPTH // 2

S5_WIDTH = D_MODEL // 2
S5_GROUP = 16
S5_GROUPS = S5_WIDTH // S5_GROUP
S5_STATE = 64
S5_LOG_DT_MIN = math.log(1e-3)
S5_LOG_DT_MAX = math.log(1e-1)
RET_WIDTH = D_MODEL // 2
RET_HEADS = 4
RET_HEAD_DIM = RET_WIDTH // RET_HEADS
RET_CHUNK = 128
ROPE_BASE = 10000.0
ROPE_PAIRS = RET_HEAD_DIM // 4
EVEN_IN = S5_WIDTH + 4 * RET_WIDTH
EVEN_MIX = S5_WIDTH + RET_WIDTH

ML_INNER = 2 * D_MODEL
ML_HEADS = 4
ML_HEAD_DIM = ML_INNER // ML_HEADS
ML_CHUNK = 128
ML_CONV_W = 5
ML_QKV_BLOCK = 4
ODD_IN = 2 * ML_INNER + 4 * ML_HEADS

kernel_name = 'hybrid_s5_retention_mlstm_prefix_dit'


def rms_norm(x, g):
    xf = x.astype(jnp.float32)
    y = xf * lax.rsqrt(jnp.mean(xf * xf, axis=-1, keepdims=True) + EPS)
    return (y * g.astype(jnp.float32)).astype(x.dtype)


def head_norm(x, g, n_heads):
    xh = x.reshape(x.shape[:-1] + (n_heads, x.shape[-1] // n_heads))
    xc = xh - jnp.mean(xh, axis=-1, keepdims=True)
    var = jnp.mean(xc * xc, axis=-1, keepdims=True)
    return (xc * lax.rsqrt(var + EPS)).reshape(x.shape) * g.astype(jnp.float32)


def modulate(h, shift, scale):
    return h * (1 + scale) + shift


def sq_relu_mlp(h, w1, w2):
    return jnp.square(jax.nn.relu(h @ w1)) @ w2


def split_heads(t, n_heads):
    b, l, w = t.shape
    return t.reshape(b, l, n_heads, w // n_heads).transpose(0, 2, 1, 3)


def merge_heads(t):
    b, h, l, dh = t.shape
    return t.transpose(0, 2, 1, 3).reshape(b, l, h * dh)


def flip_seq(t):
    return jnp.flip(t, 2)


def axial_rope(n_rows):
    row = jnp.repeat(jnp.arange(n_rows, dtype=jnp.float32), GRID_W)
    col = jnp.arange(n_rows * GRID_W) % GRID_W
    inv = ROPE_BASE ** (-jnp.arange(ROPE_PAIRS, dtype=jnp.float32) / ROPE_PAIRS)
    ang = jnp.concatenate([row[:, None] * inv, col.astype(jnp.float32)[:, None] * inv], axis=-1)
    return jnp.cos(ang), jnp.sin(ang)


def apply_rope(x, cos, sin):
    x1, x2 = jnp.split(x, 2, axis=-1)
    return jnp.concatenate([x1 * cos - x2 * sin, x1 * sin + x2 * cos], axis=-1)


def _linear_recurrence(e1, e2):
    a1, b1 = e1
    a2, b2 = e2
    return a1 * a2, a2 * b1 + b2


def s5_discretize(lam_re, lam_im, log_step, b_re, b_im):
    lam = lax.complex(lam_re.astype(jnp.float32), lam_im.astype(jnp.float32))
    step = jnp.exp(log_step.astype(jnp.float32))[:, None]
    lam_bar = jnp.exp(lam * step)
    b = lax.complex(b_re.astype(jnp.float32), b_im.astype(jnp.float32))
    b_bar = ((lam_bar - 1.0) / lam)[..., None] * b
    return lam_bar, b_bar


def s5_scan(u, lam_bar, b_bar, h0):
    bu = jnp.einsum('gpn,blgn->blgp', b_bar, u.astype(jnp.complex64))
    if h0 is not None:
        bu = bu.at[:, 0].add(lam_bar * h0)
    a = jnp.broadcast_to(lam_bar, (1, u.shape[1]) + lam_bar.shape)
    _, h = lax.associative_scan(_linear_recurrence, (a, bu), axis=1)
    return h


def s5_bidirectional(u_ctx, u_lat, lam_re, lam_im, log_step, b_re, b_im, c_re, c_im, d_skip):
    bsz = u_ctx.shape[0]
    ug_c = u_ctx.reshape(bsz, -1, S5_GROUPS, S5_GROUP)
    ug_l = u_lat.reshape(bsz, -1, S5_GROUPS, S5_GROUP)
    y_c = u_ctx * d_skip
    y_l = u_lat * d_skip
    for direction in range(2):
        lam_bar, b_bar = s5_discretize(lam_re[direction], lam_im[direction], log_step[direction],
                                       b_re[direction], b_im[direction])
        c_mat = lax.complex(c_re[direction].astype(jnp.float32), c_im[direction].astype(jnp.float32))
        seq_c = ug_c if direction == 0 else jnp.flip(ug_c, 1)
        seq_l = ug_l if direction == 0 else jnp.flip(ug_l, 1)
        h_c = s5_scan(seq_c, lam_bar, b_bar, None)
        h_l = s5_scan(seq_l, lam_bar, b_bar, h_c[:, -1])
        o_c = jnp.real(jnp.einsum('gnp,blgp->blgn', c_mat, h_c)).reshape(u_ctx.shape)
        o_l = jnp.real(jnp.einsum('gnp,blgp->blgn', c_mat, h_l)).reshape(u_lat.shape)
        if direction == 1:
            o_c = jnp.flip(o_c, 1)
            o_l = jnp.flip(o_l, 1)
        y_c = y_c + o_c
        y_l = y_l + o_l
    return y_c, y_l


def retention_scan(q, k, v, log_gamma, state0, strict):
    bsz, nh, seq, dk = q.shape
    dv = v.shape[-1]
    n_chunks = seq // RET_CHUNK

    def to_chunks(t):
        return jnp.moveaxis(t.reshape(bsz, nh, n_chunks, RET_CHUNK, t.shape[-1]), 2, 0)

    pos = jnp.arange(RET_CHUNK, dtype=jnp.float32)
    rel = pos[:, None] - pos[None, :]
    mask = rel > 0 if strict else rel >= 0
    lg = log_gamma[:, None, None]
    decay_intra = jnp.where(mask, jnp.exp(jnp.where(mask, rel, 0.0) * lg), 0.0)
    decay_query = jnp.exp((pos + 1.0) * lg[:, :, 0])[None, :, :, None]
    decay_key = jnp.exp((RET_CHUNK - 1.0 - pos) * lg[:, :, 0])[None, :, :, None]
    decay_chunk = jnp.exp(RET_CHUNK * log_gamma)[None, :, None, None]

    def step(state, inp):
        qj, kj, vj = inp
        scores = jnp.einsum('bhid,bhjd->bhij', qj, kj) * decay_intra
        out = (jnp.einsum('bhij,bhjv->bhiv', scores, vj)
               + jnp.einsum('bhid,bhdv->bhiv', qj, state) * decay_query)
        state = state * decay_chunk + jnp.einsum('bhjd,bhjv->bhdv', kj * decay_key, vj)
        return state, out

    state, out = lax.scan(step, state0, (to_chunks(q), to_chunks(k), to_chunks(v)))
    return jnp.moveaxis(out, 0, 2).reshape(bsz, nh, seq, dv), state


def retention_bidirectional(q_c, k_c, v_c, q_l, k_l, v_l, decay_logit, rope_cos, rope_sin):
    q_c, k_c, v_c, q_l, k_l, v_l = [split_heads(t, RET_HEADS) for t in (q_c, k_c, v_c, q_l, k_l, v_l)]
    q_l = apply_rope(q_l, rope_cos, rope_sin)
    k_l = apply_rope(k_l, rope_cos, rope_sin)
    k_c = k_c * RET_HEAD_DIM ** -0.5
    k_l = k_l * RET_HEAD_DIM ** -0.5
    log_gamma = jax.nn.log_sigmoid(decay_logit.astype(jnp.float32))
    zero = jnp.zeros((q_c.shape[0], RET_HEADS, RET_HEAD_DIM, RET_HEAD_DIM), jnp.float32)
    o_cf, s_cf = retention_scan(q_c, k_c, v_c, log_gamma[0], zero, False)
    o_lf, _ = retention_scan(q_l, k_l, v_l, log_gamma[0], s_cf, False)
    o_cb, s_cb = retention_scan(flip_seq(q_c), flip_seq(k_c), flip_seq(v_c), log_gamma[1], zero, True)
    o_lb, _ = retention_scan(flip_seq(q_l), flip_seq(k_l), flip_seq(v_l), log_gamma[1], s_cb, True)
    return merge_heads(o_cf + flip_seq(o_cb)), merge_heads(o_lf + flip_seq(o_lb))


def s5_retention_mixer(h_ctx, h_lat, w_in, w_out, lam_re, lam_im, log_step, b_re, b_im, c_re, c_im,
                       d_skip, w_glu, b_glu, decay_logit, gn_g, rope_cos, rope_sin, ctx_out):
    p_ctx = (h_ctx @ w_in).astype(jnp.float32)
    p_lat = (h_lat @ w_in).astype(jnp.float32)
    cuts = [S5_WIDTH + i * RET_WIDTH for i in range(4)]
    u_c, q_c, k_c, v_c, g_c = jnp.split(p_ctx, cuts, axis=-1)
    u_l, q_l, k_l, v_l, g_l = jnp.split(p_lat, cuts, axis=-1)
    s5_c, s5_l = s5_bidirectional(u_c, u_l, lam_re, lam_im, log_step, b_re, b_im, c_re, c_im, d_skip)
    ret_c, ret_l = retention_bidirectional(q_c, k_c, v_c, q_l, k_l, v_l, decay_logit, rope_cos, rope_sin)

    def combine(s5_y, ret_y, gate):
        a, b = jnp.split(jax.nn.gelu(s5_y) @ w_glu + b_glu, 2, axis=-1)
        s5_o = a * jax.nn.sigmoid(b)
        ret_o = head_norm(ret_y, gn_g, RET_HEADS) * jax.nn.silu(gate)
        return jnp.concatenate([s5_o, ret_o], axis=-1) @ w_out

    y_lat = combine(s5_l, ret_l, g_l).astype(h_lat.dtype)
    y_ctx = combine(s5_c, ret_c, g_c).astype(h_ctx.dtype) if ctx_out else None
    return y_ctx, y_lat


def centred_dwconv(x, w, b):
    width = w.shape[0]
    y = lax.conv_general_dilated(x, w.astype(x.dtype)[:, None, :], window_strides=(1,),
                                 padding=[(width // 2, width // 2)],
                                 dimension_numbers=('NWC', 'WIO', 'NWC'),
                                 feature_group_count=x.shape[-1])
    return y + b.astype(x.dtype)


def blockdiag_linear(x, w):
    bsz, seq, _ = x.shape
    xb = x.reshape(bsz, seq, w.shape[0], w.shape[1])
    return jnp.einsum('blni,nio->blno', xb, w.astype(x.dtype)).reshape(bsz, seq, -1)


def mlstm_scan(q, k, v, i_pre, log_f, state0):
    bsz, nh, seq, dk = q.shape
    dv = v.shape[-1]
    n_chunks = seq // ML_CHUNK

    def to_chunks(t):
        return jnp.moveaxis(t.reshape((bsz, nh, n_chunks, ML_CHUNK) + t.shape[3:]), 2, 0)

    tri = jnp.tril(jnp.ones((ML_CHUNK, ML_CHUNK), dtype=bool))

    def step(carry, inp):
        c_mat, n_vec, m_prev = carry
        qj, kj, vj, ij, fj = inp
        b = jnp.cumsum(fj, axis=-1)
        log_w = jnp.where(tri, b[..., :, None] - b[..., None, :] + ij[..., None, :], -jnp.inf)
        log_prev = b + m_prev[..., None]
        m_row = jnp.maximum(log_prev, jnp.max(log_w, axis=-1))
        w = jnp.exp(log_w - m_row[..., None])
        w_prev = jnp.exp(log_prev - m_row)
        s = jnp.einsum('bhid,bhjd->bhij', qj, kj) * w
        num = (jnp.einsum('bhij,bhjv->bhiv', s, vj)
               + w_prev[..., None] * jnp.einsum('bhid,bhdv->bhiv', qj, c_mat))
        den = jnp.sum(s, axis=-1) + w_prev * jnp.einsum('bhid,bhd->bhi', qj, n_vec)
        h = num / jnp.maximum(jnp.abs(den), jnp.exp(-m_row))[..., None]
        b_last = b[..., -1]
        log_k = b_last[..., None] - b + ij
        m_new = jnp.maximum(b_last + m_prev, jnp.max(log_k, axis=-1))
        w_k = jnp.exp(log_k - m_new[..., None])
        w_c = jnp.exp(b_last + m_prev - m_new)
        c_mat = w_c[..., None, None] * c_mat + jnp.einsum('bhjd,bhjv->bhdv', kj * w_k[..., None], vj)
        n_vec = w_c[..., None] * n_vec + jnp.einsum('bhj,bhjd->bhd', w_k, kj)
        return (c_mat, n_vec, m_new), h

    inputs = (to_chunks(q), to_chunks(k), to_chunks(v), to_chunks(i_pre), to_chunks(log_f))
    state, h = lax.scan(step, state0, inputs)
    return jnp.moveaxis(h, 0, 2).reshape(bsz, nh, seq, dv), state


def mlstm_mixer(h_ctx, h_lat, w_in, gate_b, conv_w, conv_b, wq, wk, wv, gn_g, skip, w_out, ctx_out):
    def prepare(h):
        p = (h @ w_in).astype(jnp.float32)
        xm, o_pre, gates = jnp.split(p, [ML_INNER, 2 * ML_INNER], axis=-1)
        xc = jax.nn.silu(centred_dwconv(xm, conv_w, conv_b))
        q = split_heads(blockdiag_linear(xc, wq), ML_HEADS)
        k = split_heads(blockdiag_linear(xc, wk), ML_HEADS) * ML_HEAD_DIM ** -0.5
        v = split_heads(blockdiag_linear(xm, wv), ML_HEADS)
        bsz, seq, _ = gates.shape
        gates = (gates + gate_b.astype(jnp.float32)).reshape(bsz, seq, 4, ML_HEADS).transpose(2, 0, 3, 1)
        return q, k, v, gates, xc, o_pre

    q_c, k_c, v_c, gt_c, xc_c, o_c = prepare(h_ctx)
    q_l, k_l, v_l, gt_l, xc_l, o_l = prepare(h_lat)
    bsz = q_c.shape[0]
    state0 = (jnp.zeros((bsz, ML_HEADS, ML_HEAD_DIM, ML_HEAD_DIM), jnp.float32),
              jnp.zeros((bsz, ML_HEADS, ML_HEAD_DIM), jnp.float32),
              jnp.zeros((bsz, ML_HEADS), jnp.float32))
    lsig = jax.nn.log_sigmoid
    hf_c, st_f = mlstm_scan(q_c, k_c, v_c, gt_c[0], lsig(gt_c[1]), state0)
    hf_l, _ = mlstm_scan(q_l, k_l, v_l, gt_l[0], lsig(gt_l[1]), st_f)
    hb_c, st_b = mlstm_scan(flip_seq(q_c), flip_seq(k_c), flip_seq(v_c),
                            flip_seq(gt_c[2]), flip_seq(lsig(gt_c[3])), state0)
    hb_l, _ = mlstm_scan(flip_seq(q_l), flip_seq(k_l), flip_seq(v_l),
                         flip_seq(gt_l[2]), flip_seq(lsig(gt_l[3])), st_b)

    def finish(h_f, h_b, xc, o_pre):
        h = merge_heads(h_f + flip_seq(h_b))
        h = head_norm(h, gn_g, ML_HEADS) + skip.astype(jnp.float32) * xc
        return (jax.nn.sigmoid(o_pre) * h) @ w_out

    y_lat = finish(hf_l, hb_l, xc_l, o_l).astype(h_lat.dtype)
    y_ctx = finish(hf_c, hb_c, xc_c, o_c).astype(h_ctx.dtype) if ctx_out else None
    return y_ctx, y_lat


def setup_inputs(seed: int = 0) -> dict:
    key = jax.random.key(seed)
    k = jax.random.split(key, 35)

    def nrm(i, shape, std):
        return std * jax.random.normal(k[i], shape, jnp.float32)

    d = D_MODEL
    g, p, n = S5_GROUPS, S5_STATE, S5_GROUP
    lam_im_base = jnp.pi * jnp.arange(p, dtype=jnp.float32)
    ret_logit_base = jnp.log(2.0 ** (5.0 + jnp.arange(RET_HEADS, dtype=jnp.float32)) - 1.0)
    f_bias = jnp.linspace(3.0, 6.0, ML_HEADS, dtype=jnp.float32)
    i_bias = jnp.zeros((ML_HEADS,), jnp.float32)
    gate_base = jnp.concatenate([i_bias, f_bias, i_bias, f_bias])
    return {
        'x': nrm(0, (BATCH, SEQ, d), 1.0),
        'c': nrm(1, (BATCH, d), 1.0),
        'ctx': nrm(2, (BATCH, CTX_LEN, d), 1.0),
        'c_ctx': nrm(3, (d,), 1.0),
        'mod_w': nrm(4, (DEPTH, d, N_MOD * d), 0.5 * d ** -0.5),
        'mod_b': nrm(5, (DEPTH, N_MOD * d), 0.02),
        'norm_mix_g': 1.0 + nrm(6, (DEPTH, d), 0.02),
        'norm_mlp_g': 1.0 + nrm(7, (DEPTH, d), 0.02),
        'mlp_w1': nrm(8, (DEPTH, d, MLP_HIDDEN), d ** -0.5),
        'mlp_w2': nrm(9, (DEPTH, MLP_HIDDEN, d), MLP_HIDDEN ** -0.5),
        'final_norm_g': 1.0 + nrm(10, (d,), 0.02),
        'ev_w_in': nrm(11, (N_EVEN, d, EVEN_IN), d ** -0.5),
        'ev_w_out': nrm(12, (N_EVEN, EVEN_MIX, d), EVEN_MIX ** -0.5),
        's5_lambda_re': -0.5 + nrm(13, (N_EVEN, 2, g, p), 0.01),
        's5_lambda_im': lam_im_base + nrm(14, (N_EVEN, 2, g, p), 0.01),
        's5_log_step': jax.random.uniform(k[15], (N_EVEN, 2, g), jnp.float32, S5_LOG_DT_MIN, S5_LOG_DT_MAX),
        's5_b_re': nrm(16, (N_EVEN, 2, g, p, n), (2 * n) ** -0.5),
        's5_b_im': nrm(17, (N_EVEN, 2, g, p, n), (2 * n) ** -0.5),
        's5_c_re': nrm(18, (N_EVEN, 2, g, n, p), (2 * p) ** -0.5),
        's5_c_im': nrm(19, (N_EVEN, 2, g, n, p), (2 * p) ** -0.5),
        's5_d': nrm(20, (N_EVEN, S5_WIDTH), 1.0),
        's5_w_glu': nrm(21, (N_EVEN, S5_WIDTH, 2 * S5_WIDTH), S5_WIDTH ** -0.5),
        's5_b_glu': nrm(22, (N_EVEN, 2 * S5_WIDTH), 0.02),
        'ret_decay_logit': ret_logit_base + nrm(23, (N_EVEN, 2, RET_HEADS), 0.01),
        'ret_gn_g': 1.0 + nrm(24, (N_EVEN, RET_WIDTH), 0.02),
        'ml_w_in': nrm(25, (N_ODD, d, ODD_IN), d ** -0.5),
        'ml_gate_b': gate_base + nrm(26, (N_ODD, 4 * ML_HEADS), 0.1),
        'ml_conv_w': nrm(27, (N_ODD, ML_CONV_W, ML_INNER), ML_CONV_W ** -0.5),
        'ml_conv_b': nrm(28, (N_ODD, ML_INNER), 0.02),
        'ml_wq': nrm(29, (N_ODD, ML_INNER // ML_QKV_BLOCK, ML_QKV_BLOCK, ML_QKV_BLOCK), ML_QKV_BLOCK ** -0.5),
        'ml_wk': nrm(30, (N_ODD, ML_INNER // ML_QKV_BLOCK, ML_QKV_BLOCK, ML_QKV_BLOCK), ML_QKV_BLOCK ** -0.5),
        'ml_wv': nrm(31, (N_ODD, ML_INNER // ML_QKV_BLOCK, ML_QKV_BLOCK, ML_QKV_BLOCK), ML_QKV_BLOCK ** -0.5),
        'ml_gn_g': 1.0 + nrm(32, (N_ODD, ML_INNER), 0.02),
        'ml_skip': 1.0 + nrm(33, (N_ODD, ML_INNER), 0.02),
        'ml_w_out': nrm(34, (N_ODD, ML_INNER, d), ML_INNER ** -0.5),
    }


def reference(x, c, ctx, c_ctx, mod_w, mod_b, norm_mix_g, norm_mlp_g, mlp_w1, mlp_w2, final_norm_g,
              ev_w_in, ev_w_out, s5_lambda_re, s5_lambda_im, s5_log_step, s5_b_re, s5_b_im, s5_c_re, s5_c_im,
              s5_d, s5_w_glu, s5_b_glu, ret_decay_logit, ret_gn_g,
              ml_w_in, ml_gate_b, ml_conv_w, ml_conv_b, ml_wq, ml_wk, ml_wv, ml_gn_g, ml_skip, ml_w_out):
    n_rows = x.shape[1] // GRID_W
    rope_cos, rope_sin = axial_rope(n_rows)
    silu_c = jax.nn.silu(c)
    silu_cc = jax.nn.silu(c_ctx)
    lat, cx = x, ctx
    for layer in range(DEPTH):
        ctx_out = layer < DEPTH - 1
        m_lat = [m[:, None, :] for m in jnp.split(silu_c @ mod_w[layer] + mod_b[layer], N_MOD, axis=-1)]
        m_ctx = jnp.split(silu_cc @ mod_w[layer] + mod_b[layer], N_MOD, axis=-1)
        h_lat = modulate(rms_norm(lat, norm_mix_g[layer]), m_lat[0], m_lat[1])
        h_ctx = modulate(rms_norm(cx, norm_mix_g[layer]), m_ctx[0], m_ctx[1])
        i = layer // 2
        if layer % 2 == 0:
            y_ctx, y_lat = s5_retention_mixer(
                h_ctx, h_lat, ev_w_in[i], ev_w_out[i], s5_lambda_re[i], s5_lambda_im[i], s5_log_step[i],
                s5_b_re[i], s5_b_im[i], s5_c_re[i], s5_c_im[i], s5_d[i], s5_w_glu[i], s5_b_glu[i],
                ret_decay_logit[i], ret_gn_g[i], rope_cos, rope_sin, ctx_out)
        else:
            y_ctx, y_lat = mlstm_mixer(
                h_ctx, h_lat, ml_w_in[i], ml_gate_b[i], ml_conv_w[i], ml_conv_b[i], ml_wq[i], ml_wk[i],
                ml_wv[i], ml_gn_g[i], ml_skip[i], ml_w_out[i], ctx_out)
        lat = lat + m_lat[2] * y_lat
        h2 = modulate(rms_norm(lat, norm_mlp_g[layer]), m_lat[3], m_lat[4])
        lat = lat + m_lat[5] * sq_relu_mlp(h2, mlp_w1[layer], mlp_w2[layer])
        if ctx_out:
            cx = cx + m_ctx[2] * y_ctx
            h2c = modulate(rms_norm(cx, norm_mlp_g[layer]), m_ctx[3], m_ctx[4])
            cx = cx + m_ctx[5] * sq_relu_mlp(h2c, mlp_w1[layer], mlp_w2[layer])
    return rms_norm(lat, final_norm_g)
```

```python
import numpy as np
from contextlib import ExitStack
import concourse.bass as bass
import concourse.mybir as mybir
from concourse.bass_utils import run_bass_kernel_spmd

F32 = mybir.dt.float32
BF16 = mybir.dt.bfloat16
AF = mybir.ActivationFunctionType
ALU = mybir.AluOpType
AX = mybir.AxisListType


class Buf:
    def __init__(self, t, name):
        self.t = t
        self.name = name
        self.lw = None
        self.rd = []
        self.dsem = None
        self.dval = 0

    def __getitem__(self, idx):
        return self.t[idx]


class KB:
    ENG = ("pe", "dve", "act", "pool", "sp")

    def __init__(self, same_engine_sync=True):
        self.nc = bass.Bass("TRN2", target_bir_lowering=False)
        nc = self.nc
        self.es = ExitStack()
        self.e = dict(pe=nc.tensor, dve=nc.vector, act=nc.scalar, pool=nc.gpsimd, sp=nc.sync)
        self.sem = {k: self.es.enter_context(nc.semaphore("sem_" + k)) for k in self.ENG}
        self.cnt = {k: 0 for k in self.ENG}
        self.seen = {k: {} for k in self.ENG}
        self.same = same_engine_sync
        self.nbuf = 0
        self.out_recs = []
        self.ninstr = 0

    def dram(self, name, shape, dtype, kind):
        return self.nc.dram_tensor(name, list(shape), dtype, kind=kind).ap()

    def sb(self, shape, dtype=F32, name=None):
        self.nbuf += 1
        name = name or f"sb{self.nbuf}"
        t = self.es.enter_context(self.nc.sbuf_tensor(name, list(shape), dtype))
        return Buf(t, name)

    def ps(self, shape, dtype=F32, name=None):
        self.nbuf += 1
        name = name or f"ps{self.nbuf}"
        t = self.es.enter_context(self.nc.psum_tensor(name, list(shape), dtype))
        return Buf(t, name)

    def _wait(self, e, rec):
        if rec is None:
            return
        kind, key, val = rec
        if kind == "eng":
            if key == e and (not self.same or e in ("pe",)):
                return
            sem = self.sem[key]
        else:
            sem = key
        sid = id(sem)
        if self.seen[e].get(sid, 0) >= val:
            return
        self.seen[e][sid] = val
        self.e[e].wait_ge(sem, val)
        self.ninstr += 1

    def _deps(self, e, reads, writes):
        for b in reads:
            self._wait(e, b.lw)
        for b in writes:
            self._wait(e, b.lw)
            for r in b.rd:
                self._wait(e, r)

    def _record(self, rec, reads, writes):
        for b in reads:
            b.rd.append(rec)
        for b in writes:
            b.lw = rec
            b.rd = []

    def op(self, e, fn, reads=(), writes=()):
        reads = [b for b in reads if isinstance(b, Buf)]
        writes = [b for b in writes if isinstance(b, Buf)]
        self._deps(e, reads, writes)
        ins = fn(self.e[e])
        self.cnt[e] += 1
        ins.then_inc(self.sem[e], 1)
        self.ninstr += 1
        self._record(("eng", e, self.cnt[e]), reads, writes)
        return ins

    def dma(self, q, out, in_, reads=(), writes=(), is_output=False):
        reads = [b for b in reads if isinstance(b, Buf)]
        writes = [b for b in writes if isinstance(b, Buf)]
        self._deps(q, reads, writes)
        owner = writes[0] if writes else reads[0]
        if owner.dsem is None:
            owner.dsem = self.es.enter_context(self.nc.semaphore("dsem_" + owner.name))
        owner.dval += 16
        self.e[q].dma_start(out=out, in_=in_).then_inc(owner.dsem, 16)
        self.ninstr += 1
        rec = ("dma", owner.dsem, owner.dval)
        self._record(rec, reads, writes)
        if is_output:
            self.out_recs.append(rec)
        return rec

    def finish(self):
        for rec in self.out_recs:
            self._wait("sp", rec)
        for k in ("pe", "dve", "act", "pool"):
            if self.cnt[k]:
                self._wait("sp", ("eng", k, self.cnt[k]))
        self.es.close()
        return self.nc


EPS = 1e-6

class Ring:
    def __init__(self, bufs):
        self.b = bufs; self.i = 0
    def next(self):
        b = self.b[self.i % len(self.b)]; self.i += 1
        return b

def load_weight_bf16(kb, Wd, K, N, name, queues=("sp", "pool")):
    KC = K // 128
    wb = kb.sb([128, KC, N], BF16, name=name)
    stg = Ring([kb.sb([128, N], F32, name=f"{name}_stg{i}") for i in range(2)])
    for k in range(KC):
        s = stg.next()
        kb.dma(queues[k % len(queues)], s[:], Wd[k * 128:(k + 1) * 128, :], writes=[s])
        if k % 2 == 0:
            kb.op("dve", lambda e: e.tensor_copy(out=wb[:, k, :], in_=s[:]), reads=[s], writes=[wb])
        else:
            kb.op("pool", lambda e: e.tensor_copy(out=wb[:, k, :], in_=s[:]), reads=[s], writes=[wb])
    return wb

def rms_rstd(kb, xt, scr, ss, rstd, D):
    kb.op("act", lambda e: e.activation(out=scr[:], in_=xt[:], func=AF.Square, accum_out=ss[:]), reads=[xt], writes=[scr, ss])
    kb.op("dve", lambda e: e.tensor_scalar(out=ss[:], in0=ss[:], scalar1=1.0 / D, scalar2=EPS, op0=ALU.mult, op1=ALU.add), reads=[ss], writes=[ss])
    kb.op("act", lambda e: e.activation(out=ss[:], in_=ss[:], func=AF.Sqrt), reads=[ss], writes=[ss])
    kb.op("dve", lambda e: e.reciprocal(out=rstd[:], in_=ss[:]), reads=[ss], writes=[rstd])

def transpose_tile(kb, src, dst, ident, pT, ncol, evac="act"):
    nk = ncol // 128
    for k0 in range(0, nk, 8):
        kk = min(8, nk - k0)
        p = pT.next()
        for k in range(kk):
            kb.op("pe", lambda e: e.transpose(out=p[:, k, :], in_=src[:, (k0 + k) * 128:(k0 + k + 1) * 128], identity=ident[:]),
                  reads=[src, ident], writes=[p])
        if evac == "act":
            kb.op("act", lambda e: e.activation(out=dst[:, k0:k0 + kk, :], in_=p[:, 0:kk, :], func=AF.Copy), reads=[p], writes=[dst])
        else:
            kb.op("dve", lambda e: e.tensor_copy(out=dst[:, k0:k0 + kk, :], in_=p[:, 0:kk, :]), reads=[p], writes=[dst])

def make_ident(kb, identd):
    idf = kb.sb([128, 128], F32, name="idf"); idb = kb.sb([128, 128], BF16, name="idb")
    kb.dma("sp", idf[:], identd[:, :], writes=[idf])
    kb.op("dve", lambda e: e.tensor_copy(out=idb[:], in_=idf[:]), reads=[idf], writes=[idb])
    return idf, idb

def build_LIN(NT, N, n_lat_tiles=16):
    D = 1024
    kb = KB()
    X = kb.dram("X", [NT * 128, D], F32, "ExternalInput")
    W = kb.dram("W", [D, N], F32, "ExternalInput")
    MOD = kb.dram("MOD", [2, 3, 128, D], F32, "ExternalInput")
    IDENT = kb.dram("IDENT", [128, 128], F32, "ExternalInput")
    O = kb.dram("O", [NT * 128, N], F32, "ExternalOutput")
    idf, idb = make_ident(kb, IDENT)
    wb = load_weight_bf16(kb, W, D, N, "wb")
    G1 = []; SH = []
    for r in range(2):
        g = kb.sb([128, D], name=f"g{r}"); sh = kb.sb([128, D], name=f"sh{r}"); sc = kb.sb([128, D], name=f"sc{r}")
        kb.dma("sp", g[:], MOD[r, 0, :, :], writes=[g]); kb.dma("sp", sh[:], MOD[r, 1, :, :], writes=[sh])
        kb.dma("sp", sc[:], MOD[r, 2, :, :], writes=[sc])
        kb.op("dve", lambda e: e.scalar_tensor_tensor(out=g[:], in0=sc[:], scalar=1.0, in1=g[:], op0=ALU.add, op1=ALU.mult), reads=[sc, g], writes=[g])
        G1.append(g); SH.append(sh)
    xr = Ring([kb.sb([128, D], name=f"x{i}") for i in range(2)])
    scr = kb.sb([128, D], name="scr")
    tmp = Ring([kb.sb([128, D], name=f"tmp{i}") for i in range(2)])
    hb = Ring([kb.sb([128, D], BF16, name=f"hb{i}") for i in range(2)])
    hT = Ring([kb.sb([128, 8, 128], BF16, name=f"hT{i}") for i in range(2)])
    ss = Ring([kb.sb([128, 1], name=f"ss{i}") for i in range(2)]); rs = Ring([kb.sb([128, 1], name=f"rs{i}") for i in range(2)])
    pT = Ring([kb.ps([128, 8, 128], BF16, name=f"pT{i}") for i in range(2)])
    pm = Ring([kb.ps([128, 512], F32, name=f"pm{i}") for i in range(4)])
    ot = Ring([kb.sb([128, N], name=f"ot{i}") for i in range(2)])
    blocks = [(s, min(512, N - s)) for s in range(0, N, 512)]
    ev = 0
    for t in range(NT):
        r = 0 if t < n_lat_tiles else 1
        x = xr.next(); s_ = ss.next(); rstd = rs.next(); tm = tmp.next(); h = hb.next(); hTt = hT.next(); o = ot.next()
        kb.dma("sp", x[:], X[t * 128:(t + 1) * 128, :], writes=[x])
        rms_rstd(kb, x, scr, s_, rstd, D)
        kb.op("dve", lambda e: e.scalar_tensor_tensor(out=tm[:], in0=x[:], scalar=rstd[:], in1=G1[r][:], op0=ALU.mult, op1=ALU.mult), reads=[x, rstd, G1[r]], writes=[tm])
        kb.op("pool", lambda e: e.tensor_tensor(out=h[:], in0=tm[:], in1=SH[r][:], op=ALU.add), reads=[tm, SH[r]], writes=[h])
        transpose_tile(kb, h, hTt, idb, pT, D)
        for (s0, w) in blocks:
            p = pm.next()
            for k in range(8):
                kb.op("pe", lambda e: e.matmul(out=p[:, 0:w], lhsT=hTt[:, k, :], rhs=wb[:, k, s0:s0 + w], start=(k == 0), stop=(k == 7)),
                      reads=[hTt, wb], writes=[p])
            if ev % 2 == 0:
                kb.op("act", lambda e: e.activation(out=o[:, s0:s0 + w], in_=p[:, 0:w], func=AF.Copy), reads=[p], writes=[o])
            else:
                kb.op("dve", lambda e: e.tensor_copy(out=o[:, s0:s0 + w], in_=p[:, 0:w]), reads=[p], writes=[o])
            ev += 1
        kb.dma("pool", O[t * 128:(t + 1) * 128, :], o[:], reads=[o], is_output=True)
    return kb.finish()

def to_tl(lat, ctx):
    out = []
    for c in range(8):
        b, q = c // 4, c % 4
        parts = [lat[b, q * 2048:(q + 1) * 2048]]
        if ctx is not None:
            pad = np.zeros((128, lat.shape[-1]), lat.dtype)
            pad[:64] = ctx[b, q * 64:(q + 1) * 64]
            parts.append(pad)
        out.append(np.ascontiguousarray(np.concatenate(parts, 0)))
    return out

def from_tl(outs, W, has_ctx=True):
    lat = np.empty((2, 8192, W), np.float32); ctx = np.empty((2, 256, W), np.float32) if has_ctx else None
    for c in range(8):
        b, q = c // 4, c % 4
        lat[b, q * 2048:(q + 1) * 2048] = outs[c][:2048]
        if has_ctx:
            ctx[b, q * 64:(q + 1) * 64] = outs[c][2048:2048 + 64]
    return lat, ctx

def rep(v):
    return np.broadcast_to(np.asarray(v, np.float32)[None, :], (128, v.shape[-1]))

def run_LIN(lat, ctx, W, g, m, layer, j_shift, j_scale):
    N = W.shape[1]
    X = to_tl(lat, ctx)
    maps = []
    for c in range(8):
        b = c // 4
        MOD = np.stack([np.stack([rep(g), rep(m[b, layer, j_shift]), rep(m[b, layer, j_scale])]),
                        np.stack([rep(g), rep(m[2, layer, j_shift]), rep(m[2, layer, j_scale])])]).astype(np.float32)
        maps.append({"X": X[c], "W": np.ascontiguousarray(W), "MOD": np.ascontiguousarray(MOD), "IDENT": np.eye(128, dtype=np.float32)})
    res = run_bass_kernel_spmd(build_LIN(17, N), maps, core_ids=list(range(8)))
    return from_tl([r["O"] for r in res.results], N)


def load_weight_bf16_q(kb, Wd, K, N, name, stg, cw=1024):
    KC = K // 128
    wb = kb.sb([128, KC, N], BF16, name=name)
    i = 0
    for k in range(KC):
        for c0 in range(0, N, cw):
            s = stg.next()
            kb.dma("sp" if i % 2 == 0 else "pool", s[:, 0:cw], Wd[k * 128:(k + 1) * 128, c0:c0 + cw], writes=[s])
            eng = "dve" if i % 2 == 0 else "pool"
            kb.op(eng, lambda e: e.tensor_copy(out=wb[:, k, c0:c0 + cw], in_=s[:, 0:cw]), reads=[s], writes=[wb])
            i += 1
    return wb

def build_MLP(NT, final, n_lat_tiles=16):
    D = 1024; H = 4096
    kb = KB()
    X = kb.dram("X", [NT * 128, D], F32, "ExternalInput")
    W1 = kb.dram("W1", [D, H], F32, "ExternalInput")
    W2 = kb.dram("W2", [H, D], F32, "ExternalInput")
    MODF = kb.dram("MODF", [2, 128, 3, 8], F32, "ExternalInput")
    GATE = kb.dram("GATE", [2, 128, D], F32, "ExternalInput")
    FG = kb.dram("FG", [128, D], F32, "ExternalInput")
    IDENT = kb.dram("IDENT", [128, 128], F32, "ExternalInput")
    O = kb.dram("O", [NT * 128, D], F32, "ExternalOutput")
    idf, idb = make_ident(kb, IDENT)
    stg = Ring([kb.sb([128, 1024], F32, name=f"stg{i}") for i in range(2)])
    w1b = load_weight_bf16_q(kb, W1, D, H, "w1b", stg)
    w2b = load_weight_bf16_q(kb, W2, H, D, "w2b", stg)
    G1 = []; SH = []; GT = []
    nvar = 2 if NT > n_lat_tiles else 1
    for r in range(nvar):
        mf = kb.sb([128, 3, 8], name=f"mf{r}"); g1 = kb.sb([128, 8], name=f"g1{r}"); gt = kb.sb([128, D], name=f"gt{r}")
        kb.dma("sp", mf[:], MODF[r, :, :, :], writes=[mf]); kb.dma("sp", gt[:], GATE[r, :, :], writes=[gt])
        kb.op("dve", lambda e: e.scalar_tensor_tensor(out=g1[:], in0=mf[:, 2, :], scalar=1.0, in1=mf[:, 0, :], op0=ALU.add, op1=ALU.mult), reads=[mf], writes=[g1])
        G1.append(g1); SH.append(mf); GT.append(gt)
    if final:
        fg = kb.sb([128, D], name="fg"); kb.dma("sp", fg[:], FG[:, :], writes=[fg])
    xr = Ring([kb.sb([128, D], name=f"x{i}") for i in range(2)])
    ot = Ring([kb.sb([128, D], name=f"ot{i}") for i in range(2)])
    xn = kb.sb([128, D], BF16, name="xn")
    hT = kb.sb([128, 8, 128], BF16, name="hT")
    hid = kb.sb([128, 32, 128], BF16, name="hid")
    rl = Ring([kb.sb([128, 512], name=f"rl{i}") for i in range(2)])
    ss = Ring([kb.sb([128, 1], name=f"ss{i}") for i in range(2)]); rs = Ring([kb.sb([128, 1], name=f"rs{i}") for i in range(2)])
    pT = Ring([kb.ps([128, 8, 128], BF16, name=f"pT{i}") for i in range(2)])
    pm = Ring([kb.ps([128, 512], F32, name=f"pm{i}") for i in range(5)])
    for t in range(NT):
        r = 0 if t < n_lat_tiles else 1
        x = xr.next(); s_ = ss.next(); rstd = rs.next(); o = ot.next()
        kb.dma("sp", x[:], X[t * 128:(t + 1) * 128, :], writes=[x])
        rms_rstd(kb, x, o, s_, rstd, D)
        kb.op("dve", lambda e: e.tensor_scalar(out=xn[:], in0=x[:], scalar1=rstd[:], scalar2=None, op0=ALU.mult), reads=[x, rstd], writes=[xn])
        p = pT.next()
        for k in range(8):
            kb.op("pe", lambda e: e.transpose(out=p[:, k, :], in_=xn[:, k * 128:(k + 1) * 128], identity=idb[:]), reads=[xn, idb], writes=[p])
        for k in range(8):
            kb.op("act", lambda e: e.activation(out=hT[:, k, :], in_=p[:, k, :], func=AF.Identity, scale=G1[r][:, k:k + 1], bias=SH[r][:, 1, k:k + 1]),
                  reads=[p, G1[r], SH[r]], writes=[hT])
        for f4 in range(8):
            pp = pm.next()
            for fi in range(4):
                f = f4 * 4 + fi
                for k in range(8):
                    kb.op("pe", lambda e: e.matmul(out=pp[:, fi * 128:(fi + 1) * 128], lhsT=w1b[:, k, f * 128:(f + 1) * 128], rhs=hT[:, k, :],
                                                  start=(k == 0), stop=(k == 7)), reads=[w1b, hT], writes=[pp])
            rr = rl.next()
            kb.op("act", lambda e: e.activation(out=rr[:], in_=pp[:], func=AF.Relu), reads=[pp], writes=[rr])
            kb.op("pool" if f4 % 2 else "dve", lambda e: e.tensor_tensor(out=hid[:, f4 * 4:(f4 + 1) * 4, :], in0=rr[:], in1=rr[:], op=ALU.mult), reads=[rr], writes=[hid])
        for cb in range(2):
            pp = pm.next()
            for f in range(32):
                kb.op("pe", lambda e: e.matmul(out=pp[:], lhsT=hid[:, f, :], rhs=w2b[:, f, cb * 512:(cb + 1) * 512], start=(f == 0), stop=(f == 31)),
                      reads=[hid, w2b], writes=[pp])
            cs = slice(cb * 512, (cb + 1) * 512)
            kb.op("dve", lambda e: e.tensor_tensor(out=o[:, cs], in0=pp[:], in1=GT[r][:, cs], op=ALU.mult), reads=[pp, GT[r]], writes=[o])
            kb.op("pool", lambda e: e.tensor_tensor(out=o[:, cs], in0=o[:, cs], in1=x[:, cs], op=ALU.add), reads=[o, x], writes=[o])
        if final:
            rms_rstd(kb, o, x, s_, rstd, D)
            kb.op("dve", lambda e: e.scalar_tensor_tensor(out=o[:], in0=o[:], scalar=rstd[:], in1=fg[:], op0=ALU.mult, op1=ALU.mult), reads=[o, rstd, fg], writes=[o])
        kb.dma("pool", O[t * 128:(t + 1) * 128, :], o[:], reads=[o], is_output=True)
    return kb.finish()

def featmaj(v):
    return np.asarray(v, np.float32).reshape(8, 128).T

def run_MLP(lat, ctx, inp, m, layer, final):
    X = to_tl(lat, ctx)
    NT = 17 if ctx is not None else 16
    g = inp["norm_mlp_g"][layer]
    maps = []
    for c in range(8):
        b = c // 4
        MODF = np.stack([np.stack([featmaj(g), featmaj(m[r, layer, 3]), featmaj(m[r, layer, 4])], 1) for r in (b, 2)]).astype(np.float32)
        GATE = np.stack([rep(m[b, layer, 5]), rep(m[2, layer, 5])]).astype(np.float32)
        maps.append({"X": X[c], "W1": inp["mlp_w1"][layer], "W2": inp["mlp_w2"][layer], "MODF": np.ascontiguousarray(MODF),
                     "GATE": np.ascontiguousarray(GATE), "FG": np.ascontiguousarray(rep(inp["final_norm_g"])), "IDENT": np.eye(128, dtype=np.float32)})
    res = run_bass_kernel_spmd(build_MLP(NT, final), maps, core_ids=list(range(8)))
    return from_tl([r["O"] for r in res.results], 1024, has_ctx=ctx is not None)


import math

L_S5 = 8448
BLK = 512
PAD = 256

def build_S5():
    kb = KB()
    U = kb.dram("U", [2, 128, L_S5], F32, "ExternalInput")
    PRM = kb.dram("PRM", [128, 3, 8], F32, "ExternalInput")
    BB = kb.dram("BB", [128, 2, 8, 16], F32, "ExternalInput")
    CC = kb.dram("CC", [128, 2, 8, 16], F32, "ExternalInput")
    IDENT = kb.dram("IDENT", [128, 128], F32, "ExternalInput")
    Y = kb.dram("Y", [2, 128, L_S5], F32, "ExternalOutput")
    idf, idb = make_ident(kb, IDENT)
    prm = kb.sb([128, 3, 8], name="prm"); bb = kb.sb([128, 2, 8, 16], name="bb"); cc = kb.sb([128, 2, 8, 16], name="cc")
    kb.dma("sp", prm[:], PRM[:, :, :], writes=[prm]); kb.dma("sp", bb[:], BB[:, :, :, :], writes=[bb]); kb.dma("sp", cc[:], CC[:, :, :, :], writes=[cc])
    ub = [kb.sb([128, L_S5], BF16, name=f"ub{d}") for d in range(2)]
    ust = Ring([kb.sb([128, 2112], name=f"ust{i}") for i in range(2)])
    for d in range(2):
        for sgi in range(4):
            s = ust.next()
            kb.dma("sp" if sgi % 2 == 0 else "pool", s[:], U[d, :, sgi * 2112:(sgi + 1) * 2112], writes=[s])
            kb.op("act", lambda e: e.activation(out=ub[d][:, sgi * 2112:(sgi + 1) * 2112], in_=s[:], func=AF.Copy), reads=[s], writes=[ub[d]])
    def T(name, shape=(128, 8)):
        return kb.sb(list(shape), name=name)
    def tt(out, a, b, op, eng="dve"):
        kb.op(eng, lambda e: e.tensor_tensor(out=out, in0=a, in1=b, op=op), reads=[prmbuf], writes=[prmbuf])
    prmbuf = Buf(None, "prmbuf")
    def vv(out_t, a, b, op):
        kb.op("dve", lambda e: e.tensor_tensor(out=out_t[:], in0=a, in1=b, op=op), reads=[prmbuf, prm, bb, cc], writes=[prmbuf])
    def act(out_t, a, func, **kw):
        kb.op("act", lambda e: e.activation(out=out_t[:], in_=a, func=func, **kw), reads=[prmbuf, prm], writes=[prmbuf])
    def ts(out_t, a, s1, s2, op0, op1=None):
        if op1 is None:
            kb.op("dve", lambda e: e.tensor_scalar(out=out_t[:], in0=a, scalar1=s1, scalar2=None, op0=op0), reads=[prmbuf, prm], writes=[prmbuf])
        else:
            kb.op("dve", lambda e: e.tensor_scalar(out=out_t[:], in0=a, scalar1=s1, scalar2=s2, op0=op0, op1=op1), reads=[prmbuf, prm], writes=[prmbuf])
    lre = prm[:, 0, :]; lim = prm[:, 1, :]; lst = prm[:, 2, :]
    step = T("step"); a_ = T("a_"); th = T("th"); ea = T("ea"); c_ = T("c_"); s_ = T("s_"); t1 = T("t1"); t2 = T("t2"); t3 = T("t3")
    hpi = T("hpi", (128, 1))
    kb.op("dve", lambda e: e.memset(hpi[:], math.pi / 2), reads=[prmbuf], writes=[prmbuf])
    act(step, lst, AF.Exp)
    vv(a_, lre, step[:], ALU.mult); vv(th, lim, step[:], ALU.mult)
    act(ea, a_[:], AF.Exp)
    act(s_, th[:], AF.Sin, scale=1.0 / 16)
    act(c_, th[:], AF.Sin, scale=1.0 / 16, bias=hpi[:])
    for _ in range(4):
        vv(t1, c_[:], c_[:], ALU.mult); vv(t2, s_[:], s_[:], ALU.mult); vv(t3, c_[:], s_[:], ALU.mult)
        vv(c_, t1[:], t2[:], ALU.subtract); ts(s_, t3[:], 2.0, None, ALU.mult)
    NLEV = 9
    PR = [T(f"pr{k}") for k in range(NLEV)]; PI = [T(f"pi{k}") for k in range(NLEV)]; NPI = [T(f"npi{k}") for k in range(NLEV)]
    vv(PR[0], ea[:], c_[:], ALU.mult); vv(PI[0], ea[:], s_[:], ALU.mult)
    for k in range(1, NLEV):
        vv(t1, PR[k - 1][:], PR[k - 1][:], ALU.mult); vv(t2, PI[k - 1][:], PI[k - 1][:], ALU.mult); vv(t3, PR[k - 1][:], PI[k - 1][:], ALU.mult)
        vv(PR[k], t1[:], t2[:], ALU.subtract); ts(PI[k], t3[:], 2.0, None, ALU.mult)
    for k in range(NLEV):
        ts(NPI[k], PI[k][:], -1.0, None, ALU.mult)
    lm1 = T("lm1"); nr = T("nr"); ni = T("ni"); den = T("den"); cr = T("cr"); ci = T("ci"); nci = T("nci")
    ts(lm1, PR[0][:], -1.0, None, ALU.add)
    vv(t1, lm1[:], lre, ALU.mult); vv(t2, PI[0][:], lim, ALU.mult); vv(nr, t1[:], t2[:], ALU.add)
    vv(t1, PI[0][:], lre, ALU.mult); vv(t2, lm1[:], lim, ALU.mult); vv(ni, t1[:], t2[:], ALU.subtract)
    vv(t1, lre, lre, ALU.mult); vv(t2, lim, lim, ALU.mult); vv(den, t1[:], t2[:], ALU.add)
    kb.op("dve", lambda e: e.reciprocal(out=den[:], in_=den[:]), reads=[prmbuf], writes=[prmbuf])
    vv(cr, nr[:], den[:], ALU.mult); vv(ci, ni[:], den[:], ALU.mult); ts(nci, ci[:], -1.0, None, ALU.mult)
    Bl = [[kb.sb([128, 128], BF16, name=f"Bl{j}_{c}") for c in range(2)] for j in range(8)]
    Cl = [[kb.sb([128, 128], BF16, name=f"Cl{j}_{c}") for c in range(2)] for j in range(8)]
    bsrc = kb.sb([128, 128], name="bsrc"); bt = T("bt", (128, 16)); bt2 = T("bt2", (128, 16))
    pTf = kb.ps([128, 128], F32, name="pTf")
    for j in range(8):
        q = j % 4
        for c in range(2):
            x1 = bb[:, 0, j, :] if c == 0 else bb[:, 1, j, :]
            x2 = bb[:, 1, j, :] if c == 0 else bb[:, 0, j, :]
            sc2 = nci if c == 0 else ci
            kb.op("dve", lambda e: e.tensor_scalar(out=bt[:], in0=x1, scalar1=cr[:, j:j + 1], scalar2=None, op0=ALU.mult), reads=[prmbuf, bb], writes=[prmbuf])
            kb.op("dve", lambda e: e.scalar_tensor_tensor(out=bt2[:], in0=x2, scalar=sc2[:, j:j + 1], in1=bt[:], op0=ALU.mult, op1=ALU.add), reads=[prmbuf, bb], writes=[prmbuf])
            kb.op("dve", lambda e: e.memset(bsrc[:], 0.0), reads=[prmbuf, bsrc], writes=[prmbuf, bsrc])
            kb.op("dve", lambda e: e.tensor_copy(out=bsrc[0:64, 32 * q:32 * q + 16], in_=bt2[0:64, :]), reads=[prmbuf, bsrc], writes=[prmbuf, bsrc])
            kb.op("dve", lambda e: e.tensor_copy(out=bsrc[64:128, 32 * q + 16:32 * q + 32], in_=bt2[64:128, :]), reads=[prmbuf, bsrc], writes=[prmbuf, bsrc])
            kb.op("pe", lambda e: e.transpose(out=pTf[:], in_=bsrc[:], identity=idf[:]), reads=[bsrc, idf], writes=[pTf])
            kb.op("act", lambda e: e.activation(out=Bl[j][c][:], in_=pTf[:], func=AF.Copy), reads=[pTf], writes=[Bl[j][c]])
            kb.op("pool", lambda e: e.memset(Cl[j][c][:], 0.0), writes=[Cl[j][c]])
            sgn = 1.0 if c == 0 else -1.0
            kb.op("act", lambda e: e.activation(out=Cl[j][c][0:64, 32 * q:32 * q + 16], in_=cc[0:64, c, j, :], func=AF.Copy, scale=sgn), reads=[cc], writes=[Cl[j][c]])
            kb.op("act", lambda e: e.activation(out=Cl[j][c][64:128, 32 * q + 16:32 * q + 32], in_=cc[64:128, c, j, :], func=AF.Copy, scale=sgn), reads=[cc], writes=[Cl[j][c]])
    W = PAD + BLK
    sets = {}
    for d in range(2):
        sets[d] = []
        for i in range(2):
            A = kb.sb([128, 2, W], name=f"A{d}{i}"); B = kb.sb([128, 2, W], name=f"B{d}{i}"); Tt = kb.sb([128, 2, BLK], name=f"T{d}{i}")
            kb.op("pool", lambda e: e.memset(A[:], 0.0), writes=[A]); kb.op("pool", lambda e: e.memset(B[:], 0.0), writes=[B])
            sets[d].append((A, B, Tt))
    hb = Ring([kb.sb([128, 2, BLK], BF16, name=f"hb{i}") for i in range(4)])
    px = Ring([kb.ps([128, 512], name=f"px{i}") for i in range(4)])
    py = Ring([kb.ps([128, 512], name=f"py{i}") for i in range(2)])
    ytr = Ring([kb.sb([128, 512], name=f"yt{i}") for i in range(4)])
    blocks = [(s0, min(BLK, L_S5 - s0)) for s0 in range(0, L_S5, BLK)]
    cnt = {0: 0, 1: 0}
    for q in range(4):
        prevs = {0: None, 1: None}
        for (s0, w) in blocks:
            st = {}
            for d in range(2):
                j = d * 4 + q
                prev = prevs[d]
                A, B, Tt = sets[d][cnt[d] % 2]; cnt[d] += 1
                pr_ = px.next(); pi_ = px.next()
                kb.op("pe", lambda e: e.matmul(out=pr_[:, 0:w], lhsT=Bl[j][0][:], rhs=ub[d][:, s0:s0 + w], start=True, stop=True), reads=[Bl[j][0], ub[d]], writes=[pr_])
                kb.op("pe", lambda e: e.matmul(out=pi_[:, 0:w], lhsT=Bl[j][1][:], rhs=ub[d][:, s0:s0 + w], start=True, stop=True), reads=[Bl[j][1], ub[d]], writes=[pi_])
                kb.op("act", lambda e: e.activation(out=A[:, 0, PAD:PAD + w], in_=pr_[:, 0:w], func=AF.Copy), reads=[pr_], writes=[A])
                kb.op("act", lambda e: e.activation(out=A[:, 1, PAD:PAD + w], in_=pi_[:, 0:w], func=AF.Copy), reads=[pi_], writes=[A])
                if prev is not None:
                    pA, pw = prev
                    cre_ = pA[:, 0, PAD + pw - 1:PAD + pw]; cim_ = pA[:, 1, PAD + pw - 1:PAD + pw]
                    a0r = A[:, 0, PAD:PAD + 1]; a0i = A[:, 1, PAD:PAD + 1]
                    kb.op("dve", lambda e: e.scalar_tensor_tensor(out=a0r, in0=cre_, scalar=PR[0][:, j:j + 1], in1=a0r, op0=ALU.mult, op1=ALU.add), reads=[pA, A, prmbuf], writes=[A])
                    kb.op("dve", lambda e: e.scalar_tensor_tensor(out=a0r, in0=cim_, scalar=NPI[0][:, j:j + 1], in1=a0r, op0=ALU.mult, op1=ALU.add), reads=[pA, A, prmbuf], writes=[A])
                    kb.op("dve", lambda e: e.scalar_tensor_tensor(out=a0i, in0=cim_, scalar=PR[0][:, j:j + 1], in1=a0i, op0=ALU.mult, op1=ALU.add), reads=[pA, A, prmbuf], writes=[A])
                    kb.op("dve", lambda e: e.scalar_tensor_tensor(out=a0i, in0=cre_, scalar=PI[0][:, j:j + 1], in1=a0i, op0=ALU.mult, op1=ALU.add), reads=[pA, A, prmbuf], writes=[A])
                st[d] = [A, B, Tt]
            nlev = int(math.ceil(math.log2(w)))
            for k in range(nlev):
                sh = 1 << k
                for d in range(2):
                    j = d * 4 + q
                    src, dst, Tt = st[d]
                    s_sh = src[:, :, PAD - sh:PAD - sh + w]
                    kb.op("dve", lambda e: e.scalar_tensor_tensor(out=Tt[:, :, 0:w], in0=s_sh, scalar=PR[k][:, j:j + 1], in1=src[:, :, PAD:PAD + w], op0=ALU.mult, op1=ALU.add),
                          reads=[src, prmbuf], writes=[Tt])
                for d in range(2):
                    j = d * 4 + q
                    src, dst, Tt = st[d]
                    kb.op("dve", lambda e: e.scalar_tensor_tensor(out=dst[:, 0, PAD:PAD + w], in0=src[:, 1, PAD - sh:PAD - sh + w], scalar=NPI[k][:, j:j + 1], in1=Tt[:, 0, 0:w], op0=ALU.mult, op1=ALU.add),
                          reads=[src, Tt, prmbuf], writes=[dst])
                for d in range(2):
                    j = d * 4 + q
                    src, dst, Tt = st[d]
                    kb.op("dve", lambda e: e.scalar_tensor_tensor(out=dst[:, 1, PAD:PAD + w], in0=src[:, 0, PAD - sh:PAD - sh + w], scalar=PI[k][:, j:j + 1], in1=Tt[:, 1, 0:w], op0=ALU.mult, op1=ALU.add),
                          reads=[src, Tt, prmbuf], writes=[dst])
                    st[d] = [dst, src, Tt]
            for d in range(2):
                j = d * 4 + q
                fin = st[d][0]
                h = hb.next()
                kb.op("act", lambda e: e.activation(out=h[:, :, 0:w], in_=fin[:, :, PAD:PAD + w], func=AF.Copy), reads=[fin], writes=[h])
                p_y = py.next()
                kb.op("pe", lambda e: e.matmul(out=p_y[:, 0:w], lhsT=Cl[j][0][:], rhs=h[:, 0, 0:w], start=True, stop=False), reads=[Cl[j][0], h], writes=[p_y])
                kb.op("pe", lambda e: e.matmul(out=p_y[:, 0:w], lhsT=Cl[j][1][:], rhs=h[:, 1, 0:w], start=False, stop=True), reads=[Cl[j][1], h], writes=[p_y])
                yt = ytr.next()
                kb.op("act", lambda e: e.activation(out=yt[32 * q:32 * q + 32, 0:w], in_=p_y[32 * q:32 * q + 32, 0:w], func=AF.Copy), reads=[p_y], writes=[yt])
                kb.dma("sp", Y[d, 32 * q:32 * q + 32, s0:s0 + w], yt[32 * q:32 * q + 32, 0:w], reads=[yt], is_output=True)
                prevs[d] = (fin, w)
    return kb.finish()

def s5_seq(lat, ctx, b, d):
    if d == 0:
        return np.concatenate([ctx[b], lat[b]], 0)
    return np.concatenate([ctx[b][::-1], lat[b][::-1]], 0)

def s5_unseq(y, d):
    c, l = y[:256], y[256:]
    if d == 1:
        c, l = c[::-1], l[::-1]
    return c, l

def run_S5(u_lat, u_ctx, inp):
    i = 0
    maps = []
    for c in range(8):
        b, gb = c // 4, c % 4
        cols = slice(gb * 128, (gb + 1) * 128)
        U = np.stack([np.ascontiguousarray(s5_seq(u_lat, u_ctx, b, d)[:, cols].T) for d in range(2)])
        PRM = np.zeros((128, 3, 8), np.float32); BB = np.zeros((128, 2, 8, 16), np.float32); CC = np.zeros((128, 2, 8, 16), np.float32)
        for j in range(8):
            d, q = j // 4, j % 4
            for ch in range(2):
                g = gb * 8 + 2 * q + ch
                ps = slice(ch * 64, (ch + 1) * 64)
                PRM[ps, 0, j] = inp["s5_lambda_re"][i, d, g]; PRM[ps, 1, j] = inp["s5_lambda_im"][i, d, g]; PRM[ps, 2, j] = inp["s5_log_step"][i, d, g]
                BB[ps, 0, j] = inp["s5_b_re"][i, d, g]; BB[ps, 1, j] = inp["s5_b_im"][i, d, g]
                CC[ps, 0, j] = inp["s5_c_re"][i, d, g].T; CC[ps, 1, j] = inp["s5_c_im"][i, d, g].T
        maps.append({"U": U.astype(np.float32), "PRM": PRM, "BB": BB, "CC": CC, "IDENT": np.eye(128, dtype=np.float32)})
    res = run_bass_kernel_spmd(build_S5(), maps, core_ids=list(range(8)))
    o_lat = np.zeros((2, 2, 8192, 512), np.float32); o_ctx = np.zeros((2, 2, 256, 512), np.float32)
    for c in range(8):
        b, gb = c // 4, c % 4
        cols = slice(gb * 128, (gb + 1) * 128)
        Y = res.results[c]["Y"]
        for d in range(2):
            cc_, ll = s5_unseq(Y[d].T, d)
            o_lat[d, b][:, cols] = ll; o_ctx[d, b][:, cols] = cc_
    return o_lat, o_ctx


NCH = 66

def build_RET():
    kb = KB()
    QKV = kb.dram("QKV", [2, L_S5, 640], F32, "ExternalInput")
    DL = kb.dram("DL", [128, 2], F32, "ExternalInput")
    POS = kb.dram("POS", [128, 1], F32, "ExternalInput")
    MASK = kb.dram("MASK", [2, 128, 128], F32, "ExternalInput")
    IDENT = kb.dram("IDENT", [128, 128], F32, "ExternalInput")
    O = kb.dram("O", [2, L_S5, 128], F32, "ExternalOutput")
    idf, idb = make_ident(kb, IDENT)
    dl = kb.sb([128, 2], name="dl"); pos = kb.sb([128, 1], name="pos")
    msk = [kb.sb([128, 128], name=f"msk{d}") for d in range(2)]
    kb.dma("sp", dl[:], DL[:, :], writes=[dl]); kb.dma("sp", pos[:], POS[:, :], writes=[pos])
    for d in range(2):
        kb.dma("sp", msk[d][:], MASK[d, :, :], writes=[msk[d]])
    lg = kb.sb([128, 2], name="lg"); aq = kb.sb([128, 2], name="aq"); ak = kb.sb([128, 2], name="ak"); g128 = kb.sb([128, 2], name="g128")
    tq = kb.sb([128, 2], name="tq")
    kb.op("act", lambda e: e.activation(out=lg[:], in_=dl[:], func=AF.Exp, scale=-1.0), reads=[dl], writes=[lg])
    kb.op("dve", lambda e: e.tensor_scalar(out=lg[:], in0=lg[:], scalar1=1.0, scalar2=None, op0=ALU.add), reads=[lg], writes=[lg])
    kb.op("act", lambda e: e.activation(out=lg[:], in_=lg[:], func=AF.Ln), reads=[lg], writes=[lg])
    kb.op("dve", lambda e: e.tensor_scalar(out=lg[:], in0=lg[:], scalar1=-1.0, scalar2=None, op0=ALU.mult), reads=[lg], writes=[lg])
    kb.op("dve", lambda e: e.tensor_scalar(out=tq[:], in0=lg[:], scalar1=pos[:, 0:1], scalar2=None, op0=ALU.mult), reads=[lg, pos], writes=[tq])
    kb.op("act", lambda e: e.activation(out=aq[:], in_=tq[:], func=AF.Exp), reads=[tq], writes=[aq])
    kb.op("act", lambda e: e.activation(out=ak[:], in_=tq[:], func=AF.Exp, scale=-1.0), reads=[tq], writes=[ak])
    kb.op("dve", lambda e: e.tensor_scalar(out=ak[:], in0=ak[:], scalar1=128.0 ** -0.5, scalar2=None, op0=ALU.mult), reads=[ak], writes=[ak])
    kb.op("act", lambda e: e.activation(out=g128[:], in_=lg[:], func=AF.Exp, scale=128.0), reads=[lg], writes=[g128])
    xin = Ring([kb.sb([128, 640], name=f"xin{i}") for i in range(6)])
    rt = Ring([kb.sb([128, 4, 2, 64], name=f"rt{i}") for i in range(4)])
    ro = Ring([kb.sb([128, 2, 2, 64], name=f"ro{i}") for i in range(4)])
    qkb = Ring([kb.sb([128, 2, 128], BF16, name=f"qkb{i}") for i in range(4)])
    vb = Ring([kb.sb([128, 128], BF16, name=f"vb{i}") for i in range(4)])
    qkT = Ring([kb.sb([128, 2, 128], BF16, name=f"qkT{i}") for i in range(4)])
    sT = Ring([kb.sb([128, 128], BF16, name=f"sT{i}") for i in range(4)])
    ot = Ring([kb.sb([128, 128], name=f"ot{i}") for i in range(6)])
    Sfs = [kb.sb([128, 128], name=f"Sf{d}") for d in range(2)]; Sbs = [kb.sb([128, 128], BF16, name=f"Sb{d}") for d in range(2)]
    pT = Ring([kb.ps([128, 8, 128], BF16, name=f"pT{i}") for i in range(2)])
    psc = Ring([kb.ps([128, 512], name=f"psc{i}") for i in range(2)])
    pso = Ring([kb.ps([128, 512], name=f"pso{i}") for i in range(2)])
    pss = Ring([kb.ps([128, 512], name=f"pss{i}") for i in range(2)])
    for d in range(2):
        kb.op("dve", lambda e: e.memset(Sfs[d][:], 0.0), writes=[Sfs[d]])
        kb.op("dve", lambda e: e.memset(Sbs[d][:], 0.0), writes=[Sbs[d]])
    for c in range(NCH):
        for d in range(2):
            Sf, Sb = Sfs[d], Sbs[d]
            x = xin.next(); t_ = rt.next(); r_ = ro.next(); qk = qkb.next(); v_ = vb.next(); qT = qkT.next(); s_ = sT.next(); o = ot.next()
            kb.dma("sp", x[:], QKV[d, c * 128:(c + 1) * 128, :], writes=[x])
            qkv = x[:, 0:256].rearrange("p (w h e) -> p w h e", w=2, h=2)
            x1 = qkv[:, :, 0, :]; x2 = qkv[:, :, 1, :]
            cs2 = x[:, 384:512].rearrange("p (w e) -> p w e", w=2); sn2 = x[:, 512:640].rearrange("p (w e) -> p w e", w=2)
            kb.op("dve", lambda e: e.tensor_tensor(out=t_[:, 0, :, :], in0=x1, in1=cs2, op=ALU.mult), reads=[x], writes=[t_])
            kb.op("pool", lambda e: e.tensor_tensor(out=t_[:, 1, :, :], in0=x2, in1=sn2, op=ALU.mult), reads=[x], writes=[t_])
            kb.op("dve", lambda e: e.tensor_tensor(out=t_[:, 2, :, :], in0=x1, in1=sn2, op=ALU.mult), reads=[x], writes=[t_])
            kb.op("pool", lambda e: e.tensor_tensor(out=t_[:, 3, :, :], in0=x2, in1=cs2, op=ALU.mult), reads=[x], writes=[t_])
            kb.op("dve", lambda e: e.tensor_tensor(out=r_[:, :, 0, :], in0=t_[:, 0, :, :], in1=t_[:, 1, :, :], op=ALU.subtract), reads=[t_], writes=[r_])
            kb.op("dve", lambda e: e.tensor_tensor(out=r_[:, :, 1, :], in0=t_[:, 2, :, :], in1=t_[:, 3, :, :], op=ALU.add), reads=[t_], writes=[r_])
            kb.op("act", lambda e: e.activation(out=qk[:, 0, :], in_=r_[:, 0, :, :], func=AF.Copy, scale=aq[:, d:d + 1]), reads=[r_, aq], writes=[qk])
            kb.op("act", lambda e: e.activation(out=qk[:, 1, :], in_=r_[:, 1, :, :], func=AF.Copy, scale=ak[:, d:d + 1]), reads=[r_, ak], writes=[qk])
            kb.op("act", lambda e: e.activation(out=v_[:], in_=x[:, 256:384], func=AF.Copy), reads=[x], writes=[v_])
            p = pT.next()
            for w in range(2):
                kb.op("pe", lambda e: e.transpose(out=p[:, w, :], in_=qk[:, w, :], identity=idb[:]), reads=[qk, idb], writes=[p])
            kb.op("dve", lambda e: e.tensor_copy(out=qT[:], in_=p[:, 0:2, :]), reads=[p], writes=[qT])
            ps_ = psc.next()
            kb.op("pe", lambda e: e.matmul(out=ps_[:, 0:128], lhsT=qT[:, 1, :], rhs=qT[:, 0, :], start=True, stop=True), reads=[qT], writes=[ps_])
            kb.op("dve", lambda e: e.tensor_tensor(out=s_[:], in0=ps_[:, 0:128], in1=msk[d][:], op=ALU.mult), reads=[ps_, msk[d]], writes=[s_])
            po = pso.next()
            kb.op("pe", lambda e: e.matmul(out=po[:, 0:128], lhsT=s_[:], rhs=v_[:], start=True, stop=False), reads=[s_, v_], writes=[po])
            kb.op("pe", lambda e: e.matmul(out=po[:, 0:128], lhsT=qT[:, 0, :], rhs=Sb[:], start=False, stop=True), reads=[qT, Sb], writes=[po])
            kb.op("act", lambda e: e.activation(out=o[:], in_=po[:, 0:128], func=AF.Copy), reads=[po], writes=[o])
            kb.dma("act", O[d, c * 128:(c + 1) * 128, :], o[:], reads=[o], is_output=True)
            pS = pss.next()
            kb.op("pe", lambda e: e.matmul(out=pS[:, 0:128], lhsT=qk[:, 1, :], rhs=v_[:], start=True, stop=True), reads=[qk, v_], writes=[pS])
            kb.op("dve", lambda e: e.tensor_scalar(out=Sf[:], in0=Sf[:], scalar1=g128[:, d:d + 1], scalar2=None, op0=ALU.mult), reads=[Sf, g128], writes=[Sf])
            kb.op("dve", lambda e: e.scalar_tensor_tensor(out=Sf[:], in0=pS[:, 0:128], scalar=g128[:, d:d + 1], in1=Sf[:], op0=ALU.mult, op1=ALU.add), reads=[pS, Sf, g128], writes=[Sf])
            kb.op("act", lambda e: e.activation(out=Sb[:], in_=Sf[:], func=AF.Copy), reads=[Sf], writes=[Sb])
    return kb.finish()

def rope_tables():
    n_rows = 8192 // 64
    row = np.repeat(np.arange(n_rows, dtype=np.float32), 64)
    col = (np.arange(8192) % 64).astype(np.float32)
    inv = (10000.0 ** (-np.arange(32, dtype=np.float32) / 32)).astype(np.float32)
    ang = np.concatenate([row[:, None] * inv, col[:, None] * inv], axis=-1)
    return np.cos(ang).astype(np.float32), np.sin(ang).astype(np.float32)

def run_RET(p_lat, p_ctx, inp):
    cos, sin = rope_tables()
    cos_c = np.ones((256, 64), np.float32); sin_c = np.zeros((256, 64), np.float32)
    maps = []
    for c in range(8):
        b, h = c // 4, c % 4
        seqs = []
        for d in range(2):
            def sl(off):
                cols = slice(off + h * 128, off + (h + 1) * 128)
                return s5_seq(p_lat[:, :, cols], p_ctx[:, :, cols], b, d)
            cs = s5_seq(cos[None].repeat(2, 0), cos_c[None].repeat(2, 0), b, d)
            sn = s5_seq(sin[None].repeat(2, 0), sin_c[None].repeat(2, 0), b, d)
            seqs.append(np.concatenate([sl(512), sl(1024), sl(1536), cs, cs, sn, sn], axis=1))
        QKV = np.ascontiguousarray(np.stack(seqs)).astype(np.float32)
        DL = np.ascontiguousarray(np.broadcast_to(inp["ret_decay_logit"][0][:, h][None, :], (128, 2))).astype(np.float32)
        POS = (np.arange(128, dtype=np.float32) + 1)[:, None]
        jj, ii = np.meshgrid(np.arange(128), np.arange(128), indexing="ij")
        MASK = np.stack([(jj <= ii), (jj < ii)]).astype(np.float32)
        maps.append({"QKV": QKV, "DL": DL, "POS": POS, "MASK": MASK, "IDENT": np.eye(128, dtype=np.float32)})
    res = run_bass_kernel_spmd(build_RET(), maps, core_ids=list(range(8)))
    o_lat = np.zeros((2, 2, 8192, 512), np.float32); o_ctx = np.zeros((2, 2, 256, 512), np.float32)
    for c in range(8):
        b, h = c // 4, c % 4
        cols = slice(h * 128, (h + 1) * 128)
        Y = res.results[c]["O"]
        for d in range(2):
            cc_, ll = s5_unseq(Y[d], d)
            o_lat[d, b][:, cols] = ll; o_ctx[d, b][:, cols] = cc_
    return o_lat, o_ctx


def build_C0(NT=17, n_lat_tiles=16):
    D = 1024
    kb = KB()
    S5F = kb.dram("S5F", [NT * 128, 512], F32, "ExternalInput"); S5B = kb.dram("S5B", [NT * 128, 512], F32, "ExternalInput")
    RTF = kb.dram("RTF", [NT * 128, 512], F32, "ExternalInput"); RTB = kb.dram("RTB", [NT * 128, 512], F32, "ExternalInput")
    UU = kb.dram("UU", [NT * 128, 512], F32, "ExternalInput"); GG = kb.dram("GG", [NT * 128, 512], F32, "ExternalInput")
    X = kb.dram("X", [NT * 128, D], F32, "ExternalInput")
    WG = kb.dram("WG", [512, 1024], F32, "ExternalInput"); WO = kb.dram("WO", [1024, 1024], F32, "ExternalInput")
    REP = kb.dram("REP", [4, 128, 1024], F32, "ExternalInput")
    IDENT = kb.dram("IDENT", [128, 128], F32, "ExternalInput")
    O = kb.dram("O", [NT * 128, D], F32, "ExternalOutput")
    idf, idb = make_ident(kb, IDENT)
    stg = Ring([kb.sb([128, 1024], F32, name=f"stg{i}") for i in range(2)])
    wg = load_weight_bf16_q(kb, WG, 512, 1024, "wg", stg)
    wo = load_weight_bf16_q(kb, WO, 1024, 1024, "wo", stg)
    reps = []
    for i in range(4):
        r = kb.sb([128, 1024], name=f"rep{i}"); kb.dma("sp", r[:], REP[i, :, :], writes=[r]); reps.append(r)
    bglu, dgn, gate = reps[0], reps[1], (reps[2], reps[3])
    def ring(n, shape, dt=F32, nm="r"):
        return Ring([kb.sb(list(shape), dt, name=f"{nm}{i}") for i in range(n)])
    i_s5f = ring(2, [128, 512], nm="is5f"); i_s5b = ring(2, [128, 512], nm="is5b"); i_rf = ring(2, [128, 512], nm="irf"); i_rb = ring(2, [128, 512], nm="irb")
    i_u = ring(2, [128, 512], nm="iu"); i_g = ring(2, [128, 512], nm="ig"); i_x = ring(2, [128, D], nm="ix")
    y = kb.sb([128, 512], name="y"); w_ = kb.sb([128, 512], name="w_"); sg = kb.sb([128, 512], name="sg")
    ge = kb.sb([128, 512], BF16, name="ge"); geT = kb.sb([128, 4, 128], BF16, name="geT")
    a_ = kb.sb([128, 512], name="a_"); bg = kb.sb([128, 512], name="bg")
    cat = kb.sb([128, 1024], BF16, name="cat"); catT = kb.sb([128, 8, 128], BF16, name="catT")
    rr = kb.sb([128, 4, 128], name="rr"); xn = kb.sb([128, 4, 128], name="xn"); sq = kb.sb([128, 128], name="sq")
    sm = kb.sb([128, 4], name="sm"); s2 = kb.sb([128, 4], name="s2"); mn = kb.sb([128, 4], name="mn"); vr = kb.sb([128, 4], name="vr"); rsd = kb.sb([128, 4], name="rsd")
    slg = kb.sb([128, 512], name="slg")
    ot = ring(2, [128, D], nm="ot")
    pT = Ring([kb.ps([128, 8, 128], BF16, name=f"pT{i}") for i in range(2)])
    pm = Ring([kb.ps([128, 512], F32, name=f"pm{i}") for i in range(4)])
    for t in range(NT):
        rws = slice(t * 128, (t + 1) * 128)
        r = 0 if t < n_lat_tiles else 1
        s5f = i_s5f.next(); s5b = i_s5b.next(); rf = i_rf.next(); rb = i_rb.next(); u = i_u.next(); g = i_g.next(); x = i_x.next(); o = ot.next()
        for (buf, src) in ((s5f, S5F), (s5b, S5B), (rf, RTF), (rb, RTB), (u, UU), (g, GG), (x, X)):
            kb.dma("sp", buf[:], src[rws, :], writes=[buf])
        kb.op("dve", lambda e: e.tensor_tensor(out=y[:], in0=s5f[:], in1=s5b[:], op=ALU.add), reads=[s5f, s5b], writes=[y])
        kb.op("pool", lambda e: e.tensor_tensor(out=w_[:], in0=u[:], in1=dgn[:, 0:512], op=ALU.mult), reads=[u, dgn], writes=[w_])
        kb.op("dve", lambda e: e.tensor_tensor(out=y[:], in0=y[:], in1=w_[:], op=ALU.add), reads=[y, w_], writes=[y])
        kb.op("dve", lambda e: e.tensor_tensor(out=w_[:], in0=y[:], in1=y[:], op=ALU.mult), reads=[y], writes=[w_])
        kb.op("dve", lambda e: e.tensor_scalar(out=w_[:], in0=w_[:], scalar1=0.044715, scalar2=1.0, op0=ALU.mult, op1=ALU.add), reads=[w_], writes=[w_])
        kb.op("dve", lambda e: e.tensor_tensor(out=w_[:], in0=w_[:], in1=y[:], op=ALU.mult), reads=[w_, y], writes=[w_])
        kb.op("act", lambda e: e.activation(out=sg[:], in_=w_[:], func=AF.Sigmoid, scale=1.5957691216057308), reads=[w_], writes=[sg])
        kb.op("dve", lambda e: e.tensor_tensor(out=ge[:], in0=sg[:], in1=y[:], op=ALU.mult), reads=[sg, y], writes=[ge])
        transpose_tile(kb, ge, geT, idb, pT, 512)
        pa = pm.next(); pb = pm.next()
        for (pp, c0) in ((pa, 0), (pb, 512)):
            for k in range(4):
                kb.op("pe", lambda e: e.matmul(out=pp[:], lhsT=geT[:, k, :], rhs=wg[:, k, c0:c0 + 512], start=(k == 0), stop=(k == 3)), reads=[geT, wg], writes=[pp])
        kb.op("dve", lambda e: e.tensor_tensor(out=a_[:], in0=pa[:], in1=bglu[:, 0:512], op=ALU.add), reads=[pa, bglu], writes=[a_])
        kb.op("dve", lambda e: e.tensor_tensor(out=bg[:], in0=pb[:], in1=bglu[:, 512:1024], op=ALU.add), reads=[pb, bglu], writes=[bg])
        kb.op("act", lambda e: e.activation(out=bg[:], in_=bg[:], func=AF.Sigmoid), reads=[bg], writes=[bg])
        kb.op("dve", lambda e: e.tensor_tensor(out=cat[:, 0:512], in0=a_[:], in1=bg[:], op=ALU.mult), reads=[a_, bg], writes=[cat])
        rr2 = rr[:].rearrange("p h e -> p (h e)")
        kb.op("dve", lambda e: e.tensor_tensor(out=rr2, in0=rf[:], in1=rb[:], op=ALU.add), reads=[rf, rb], writes=[rr])
        kb.op("dve", lambda e: e.tensor_reduce(out=sm[:], in_=rr[:], axis=AX.X, op=ALU.add), reads=[rr], writes=[sm])
        for h in range(4):
            kb.op("act", lambda e: e.activation(out=sq[:], in_=rr[:, h, :], func=AF.Square, accum_out=s2[:, h:h + 1]), reads=[rr], writes=[sq, s2])
        kb.op("dve", lambda e: e.tensor_scalar(out=mn[:], in0=sm[:], scalar1=1.0 / 128, scalar2=None, op0=ALU.mult), reads=[sm], writes=[mn])
        kb.op("dve", lambda e: e.tensor_tensor(out=vr[:], in0=mn[:], in1=mn[:], op=ALU.mult), reads=[mn], writes=[vr])
        kb.op("dve", lambda e: e.scalar_tensor_tensor(out=vr[:], in0=s2[:], scalar=1.0 / 128, in1=vr[:], op0=ALU.mult, op1=ALU.subtract), reads=[s2, vr], writes=[vr])
        kb.op("dve", lambda e: e.tensor_scalar(out=vr[:], in0=vr[:], scalar1=EPS, scalar2=None, op0=ALU.add), reads=[vr], writes=[vr])
        kb.op("act", lambda e: e.activation(out=vr[:], in_=vr[:], func=AF.Sqrt), reads=[vr], writes=[vr])
        kb.op("dve", lambda e: e.reciprocal(out=rsd[:], in_=vr[:]), reads=[vr], writes=[rsd])
        for h in range(4):
            kb.op("dve", lambda e: e.tensor_scalar(out=xn[:, h, :], in0=rr[:, h, :], scalar1=mn[:, h:h + 1], scalar2=rsd[:, h:h + 1], op0=ALU.subtract, op1=ALU.mult),
                  reads=[rr, mn, rsd], writes=[xn])
        xn2 = xn[:].rearrange("p h e -> p (h e)")
        kb.op("act", lambda e: e.activation(out=slg[:], in_=g[:], func=AF.Silu), reads=[g], writes=[slg])
        kb.op("pool", lambda e: e.tensor_tensor(out=xn2, in0=xn2, in1=dgn[:, 512:1024], op=ALU.mult), reads=[xn, dgn], writes=[xn])
        kb.op("dve", lambda e: e.tensor_tensor(out=cat[:, 512:1024], in0=xn2, in1=slg[:], op=ALU.mult), reads=[xn, slg], writes=[cat])
        transpose_tile(kb, cat, catT, idb, pT, 1024)
        for cb in range(2):
            pp = pm.next(); cs = slice(cb * 512, (cb + 1) * 512)
            for k in range(8):
                kb.op("pe", lambda e: e.matmul(out=pp[:], lhsT=catT[:, k, :], rhs=wo[:, k, cs], start=(k == 0), stop=(k == 7)), reads=[catT, wo], writes=[pp])
            kb.op("dve", lambda e: e.tensor_tensor(out=o[:, cs], in0=pp[:], in1=gate[r][:, cs], op=ALU.mult), reads=[pp, gate[r]], writes=[o])
            kb.op("pool", lambda e: e.tensor_tensor(out=o[:, cs], in0=o[:, cs], in1=x[:, cs], op=ALU.add), reads=[o, x], writes=[o])
        kb.dma("act", O[rws, :], o[:], reads=[o], is_output=True)
    return kb.finish()

def run_C0(lat, ctx, s5f, s5b, rtf, rtb, p_lat, p_ctx, inp, m):
    tl = lambda a: to_tl(a[0], a[1])
    S5F, S5B, RTF, RTB = tl(s5f), tl(s5b), tl(rtf), tl(rtb)
    UU = to_tl(p_lat[:, :, 0:512], p_ctx[:, :, 0:512]); GG = to_tl(p_lat[:, :, 2048:2560], p_ctx[:, :, 2048:2560])
    X = to_tl(lat, ctx)
    maps = []
    for c in range(8):
        b = c // 4
        REP = np.stack([rep(inp["s5_b_glu"][0]), rep(np.concatenate([inp["s5_d"][0], inp["ret_gn_g"][0]])), rep(m[b, 0, 2]), rep(m[2, 0, 2])]).astype(np.float32)
        maps.append({"S5F": S5F[c], "S5B": S5B[c], "RTF": RTF[c], "RTB": RTB[c], "UU": UU[c], "GG": GG[c], "X": X[c],
                     "WG": inp["s5_w_glu"][0], "WO": inp["ev_w_out"][0], "REP": np.ascontiguousarray(REP), "IDENT": np.eye(128, dtype=np.float32)})
    res = run_bass_kernel_spmd(build_C0(), maps, core_ids=list(range(8)))
    return from_tl([r["O"] for r in res.results], 1024)


LP = 8456
LO = 8452

def build_QKV1():
    kb = KB()
    XM = kb.dram("XM", [4, 128, LP], F32, "ExternalInput")
    CW = kb.dram("CW", [128, 4, 6], F32, "ExternalInput")
    BD = kb.dram("BD", [3, 4, 128, 128], F32, "ExternalInput")
    OUT = kb.dram("OUT", [4, 4, 128, LO], F32, "ExternalOutput")
    cw = kb.sb([128, 4, 6], name="cw"); kb.dma("sp", cw[:], CW[:, :, :], writes=[cw])
    bdf = kb.sb([128, 12, 128], name="bdf"); bdb = kb.sb([128, 12, 128], BF16, name="bdb")
    for w in range(3):
        for t in range(4):
            kb.dma("sp", bdf[:, w * 4 + t, :], BD[w, t, :, :], writes=[bdf])
    kb.op("dve", lambda e: e.tensor_copy(out=bdb[:], in_=bdf[:]), reads=[bdf], writes=[bdb])
    xin = Ring([kb.sb([128, 516], name=f"xin{i}") for i in range(3)])
    acc = Ring([kb.sb([128, 512], name=f"acc{i}") for i in range(2)])
    xcf = Ring([kb.sb([128, 512], name=f"xcf{i}") for i in range(3)])
    xcb = Ring([kb.sb([128, 512], BF16, name=f"xcb{i}") for i in range(2)])
    xmb = Ring([kb.sb([128, 512], BF16, name=f"xmb{i}") for i in range(2)])
    oq = Ring([kb.sb([128, 3, 512], name=f"oq{i}") for i in range(2)])
    pm = Ring([kb.ps([128, 512], name=f"pm{i}") for i in range(6)])
    blocks = [(s0, min(512, LO - s0)) for s0 in range(0, LO, 512)]
    for t in range(4):
        for (s0, w) in blocks:
            x = xin.next(); a = acc.next(); xf = xcf.next(); xb = xcb.next(); mb = xmb.next(); o = oq.next()
            kb.dma("sp", x[:, 0:w + 4], XM[t, :, s0:s0 + w + 4], writes=[x])
            kb.op("dve", lambda e: e.tensor_scalar(out=a[:, 0:w], in0=x[:, 0:w], scalar1=cw[:, t, 0:1], scalar2=None, op0=ALU.mult), reads=[x, cw], writes=[a])
            for k in range(1, 5):
                kb.op("dve", lambda e: e.scalar_tensor_tensor(out=a[:, 0:w], in0=x[:, k:k + w], scalar=cw[:, t, k:k + 1], in1=a[:, 0:w], op0=ALU.mult, op1=ALU.add),
                      reads=[x, cw, a], writes=[a])
            kb.op("act", lambda e: e.activation(out=xf[:, 0:w], in_=a[:, 0:w], func=AF.Silu, bias=cw[:, t, 5:6]), reads=[a, cw], writes=[xf])
            kb.op("pool", lambda e: e.tensor_copy(out=xb[:, 0:w], in_=xf[:, 0:w]), reads=[xf], writes=[xb])
            kb.op("pool", lambda e: e.tensor_copy(out=mb[:, 0:w], in_=x[:, 2:2 + w]), reads=[x], writes=[mb])
            for wi in range(3):
                p = pm.next()
                src = mb if wi == 2 else xb
                kb.op("pe", lambda e: e.matmul(out=p[:, 0:w], lhsT=bdb[:, wi * 4 + t, :], rhs=src[:, 0:w], start=True, stop=True), reads=[bdb, src], writes=[p])
                kb.op("act", lambda e: e.activation(out=o[:, wi, 0:w], in_=p[:, 0:w], func=AF.Copy), reads=[p], writes=[o])
            for wi in range(3):
                kb.dma("act", OUT[wi, t, :, s0:s0 + w], o[:, wi, 0:w], reads=[o], is_output=True)
            kb.dma("act", OUT[3, t, :, s0:s0 + w], xf[:, 0:w], reads=[xf], is_output=True)
    return kb.finish()

def pad_nat(ctx_a, lat_a):
    C = lat_a.shape[1]
    z2 = np.zeros((2, C), np.float32)
    return np.concatenate([z2, ctx_a, z2, z2, lat_a, z2], 0)

def run_QKV1(p_lat, p_ctx, inp):
    maps = []
    for c in range(8):
        b, h = c // 4, c % 4
        cols = slice(h * 512, (h + 1) * 512)
        xp = pad_nat(p_ctx[b][:, cols], p_lat[b][:, cols])
        XM = np.ascontiguousarray(xp.T.reshape(4, 128, LP))
        CW = np.zeros((128, 4, 6), np.float32)
        cwf = inp["ml_conv_w"][0][:, cols]
        CW[:, :, 0:5] = cwf.T.reshape(4, 128, 5).transpose(1, 0, 2)
        CW[:, :, 5] = inp["ml_conv_b"][0][cols].reshape(4, 128).T
        BD = np.zeros((3, 4, 128, 128), np.float32)
        for wi, nm in enumerate(("ml_wq", "ml_wk", "ml_wv")):
            wblk = inp[nm][0][h * 128:(h + 1) * 128]
            for t in range(4):
                for nb in range(32):
                    BD[wi, t, 4 * nb:4 * nb + 4, 4 * nb:4 * nb + 4] = wblk[t * 32 + nb]
        maps.append({"XM": XM, "CW": CW, "BD": BD})
    res = run_bass_kernel_spmd(build_QKV1(), maps, core_ids=list(range(8)))
    outs = [(np.zeros((2, 8192, 2048), np.float32), np.zeros((2, 256, 2048), np.float32)) for _ in range(4)]
    for c in range(8):
        b, h = c // 4, c % 4
        cols = slice(h * 512, (h + 1) * 512)
        O = res.results[c]["OUT"]
        for wi in range(4):
            full = O[wi].reshape(512, LO).T
            outs[wi][1][b][:, cols] = full[0:256]
            outs[wi][0][b][:, cols] = full[260:260 + 8192]
    return outs

def build_ML():
    kb = KB()
    QKV = kb.dram("QKV", [2, L_S5, 1538], F32, "ExternalInput")
    GB = kb.dram("GB", [128, 4], F32, "ExternalInput")
    TRI = kb.dram("TRI", [2, 128, 128], F32, "ExternalInput")
    IDENT = kb.dram("IDENT", [128, 128], F32, "ExternalInput")
    O = kb.dram("O", [2, L_S5, 512], F32, "ExternalOutput")
    idf, idb = make_ident(kb, IDENT)
    gb = kb.sb([128, 4], name="gb"); kb.dma("sp", gb[:], GB[:, :], writes=[gb])
    tri = kb.sb([128, 128], name="tri"); ones = kb.sb([128, 128], name="ones")
    kb.dma("sp", tri[:], TRI[0, :, :], writes=[tri]); kb.dma("sp", ones[:], TRI[1, :, :], writes=[ones])
    onesb = kb.sb([128, 1], BF16, name="onesb")
    kb.op("dve", lambda e: e.tensor_copy(out=onesb[:], in_=ones[:, 0:1]), reads=[ones], writes=[onesb])
    gbn = kb.sb([128, 4], name="gbn"); gbi = kb.sb([128, 4], name="gbi")
    kb.op("dve", lambda e: e.tensor_scalar(out=gbn[:], in0=gb[:], scalar1=-1.0, scalar2=None, op0=ALU.mult), reads=[gb], writes=[gbn])
    kb.op("dve", lambda e: e.tensor_scalar(out=gbi[:], in0=gb[:], scalar1=float(np.log(512.0 ** -0.5)), scalar2=None, op0=ALU.add), reads=[gb], writes=[gbi])
    xin = Ring([kb.sb([128, 1538], name=f"xin{i}") for i in range(4)])
    sm = Ring([kb.sb([128, 8], name=f"sm{i}") for i in range(4)])
    qb = Ring([kb.sb([128, 512], BF16, name=f"qb{i}") for i in range(4)])
    kbf = Ring([kb.sb([128, 512], BF16, name=f"kbf{i}") for i in range(4)])
    vb = Ring([kb.sb([128, 512], BF16, name=f"vb{i}") for i in range(4)])
    qkT = Ring([kb.sb([128, 8, 128], BF16, name=f"qkT{i}") for i in range(4)])
    sT = Ring([kb.sb([128, 128], BF16, name=f"sT{i}") for i in range(4)])
    ot = Ring([kb.sb([128, 512], name=f"ot{i}") for i in range(4)])
    Cfs = [kb.sb([128, 4, 512], name=f"Cf{d}") for d in range(2)]; Cbs = [kb.sb([128, 4, 512], BF16, name=f"Cb{d}") for d in range(2)]
    nfs = [kb.sb([128, 4], name=f"nf{d}") for d in range(2)]; nbs = [kb.sb([128, 4], BF16, name=f"nb{d}") for d in range(2)]
    pT = Ring([kb.ps([128, 8, 128], BF16, name=f"pT{i}") for i in range(1)])
    psm = Ring([kb.ps([128, 512], name=f"psm{i}") for i in range(1)])
    psc = Ring([kb.ps([128, 512], name=f"psc{i}") for i in range(1)])
    pnum = Ring([kb.ps([128, 512], name=f"pnum{i}") for i in range(1)])
    pden = Ring([kb.ps([128, 512], name=f"pden{i}") for i in range(1)])
    pst = Ring([kb.ps([128, 512], name=f"pst{i}") for i in range(2)])
    pn = Ring([kb.ps([128, 512], name=f"pn{i}") for i in range(1)])
    for d in range(2):
        Cf, Cb, nf, nb_ = Cfs[d], Cbs[d], nfs[d], nbs[d]
        kb.op("dve", lambda e: e.memset(Cf[:], 0.0), writes=[Cf]); kb.op("pool", lambda e: e.memset(Cb[:], 0.0), writes=[Cb])
        kb.op("dve", lambda e: e.memset(nf[:], 0.0), writes=[nf]); kb.op("pool", lambda e: e.memset(nb_[:], 0.0), writes=[nb_])
    for c in range(NCH):
        for d in range(2):
            Cf, Cb, nf, nb_ = Cfs[d], Cbs[d], nfs[d], nbs[d]
            x = xin.next(); s = sm.next(); q_ = qb.next(); k_ = kbf.next(); v_ = vb.next(); T_ = qkT.next(); s_ = sT.next(); o = ot.next()
            kb.dma("sp", x[:], QKV[d, c * 128:(c + 1) * 128, :], writes=[x])
            kb.op("act", lambda e: e.activation(out=s[:, 0:1], in_=x[:, 1537:1538], func=AF.Exp, scale=-1.0, bias=gbn[:, 2 * d + 1:2 * d + 2]), reads=[x, gbn], writes=[s])
            kb.op("act", lambda e: e.activation(out=s[:, 0:1], in_=s[:, 0:1], func=AF.Ln, bias=ones[:, 0:1]), reads=[s, ones], writes=[s])
            kb.op("dve", lambda e: e.tensor_tensor(out=s[:, 1:2], in0=x[:, 1536:1537], in1=gbi[:, 2 * d:2 * d + 1], op=ALU.add), reads=[x, gbi], writes=[s])
            pc = psm.next()
            kb.op("pe", lambda e: e.matmul(out=pc[:, 0:2], lhsT=tri[:], rhs=s[:, 0:2], start=True, stop=True), reads=[tri, s], writes=[pc])
            kb.op("pe", lambda e: e.matmul(out=pc[:, 2:4], lhsT=ones[:], rhs=s[:, 0:2], start=True, stop=True), reads=[ones, s], writes=[pc])
            kb.op("act", lambda e: e.activation(out=s[:, 2:3], in_=pc[:, 0:1], func=AF.Exp, scale=-1.0), reads=[pc], writes=[s])
            kb.op("act", lambda e: e.activation(out=s[:, 3:4], in_=pc[:, 0:1], func=AF.Exp, bias=s[:, 1:2]), reads=[pc, s], writes=[s])
            kb.op("act", lambda e: e.activation(out=s[:, 4:5], in_=pc[:, 2:3], func=AF.Exp, scale=-1.0), reads=[pc], writes=[s])
            kb.op("act", lambda e: e.activation(out=q_[:], in_=x[:, 0:512], func=AF.Copy, scale=s[:, 2:3]), reads=[x, s], writes=[q_])
            kb.op("act", lambda e: e.activation(out=k_[:], in_=x[:, 512:1024], func=AF.Copy, scale=s[:, 3:4]), reads=[x, s], writes=[k_])
            kb.op("pool", lambda e: e.tensor_copy(out=v_[:], in_=x[:, 1024:1536]), reads=[x], writes=[v_])
            p = pT.next()
            for dt_ in range(4):
                kb.op("pe", lambda e: e.transpose(out=p[:, dt_, :], in_=q_[:, dt_ * 128:(dt_ + 1) * 128], identity=idb[:]), reads=[q_, idb], writes=[p])
            for dt_ in range(4):
                kb.op("pe", lambda e: e.transpose(out=p[:, 4 + dt_, :], in_=k_[:, dt_ * 128:(dt_ + 1) * 128], identity=idb[:]), reads=[k_, idb], writes=[p])
            kb.op("dve", lambda e: e.tensor_copy(out=T_[:], in_=p[:]), reads=[p], writes=[T_])
            ps_ = psc.next()
            for dt_ in range(4):
                kb.op("pe", lambda e: e.matmul(out=ps_[:, 0:128], lhsT=T_[:, 4 + dt_, :], rhs=T_[:, dt_, :], start=(dt_ == 0), stop=(dt_ == 3)), reads=[T_], writes=[ps_])
            kb.op("dve", lambda e: e.tensor_tensor(out=s_[:], in0=ps_[:, 0:128], in1=tri[:], op=ALU.mult), reads=[ps_, tri], writes=[s_])
            pn_ = pnum.next(); pd = pden.next()
            kb.op("pe", lambda e: e.matmul(out=pn_[:], lhsT=s_[:], rhs=v_[:], start=True, stop=False), reads=[s_, v_], writes=[pn_])
            for dt_ in range(4):
                kb.op("pe", lambda e: e.matmul(out=pn_[:], lhsT=T_[:, dt_, :], rhs=Cb[:, dt_, :], start=False, stop=(dt_ == 3)), reads=[T_, Cb], writes=[pn_])
            kb.op("pe", lambda e: e.matmul(out=pd[:, 0:1], lhsT=s_[:], rhs=onesb[:], start=True, stop=False), reads=[s_, onesb], writes=[pd])
            for dt_ in range(4):
                kb.op("pe", lambda e: e.matmul(out=pd[:, 0:1], lhsT=T_[:, dt_, :], rhs=nb_[:, dt_:dt_ + 1], start=False, stop=(dt_ == 3)), reads=[T_, nb_], writes=[pd])
            kb.op("act", lambda e: e.activation(out=s[:, 5:6], in_=pd[:, 0:1], func=AF.Abs), reads=[pd], writes=[s])
            kb.op("dve", lambda e: e.tensor_scalar(out=s[:, 5:6], in0=s[:, 5:6], scalar1=1.0, scalar2=None, op0=ALU.max), reads=[s], writes=[s])
            kb.op("dve", lambda e: e.reciprocal(out=s[:, 6:7], in_=s[:, 5:6]), reads=[s], writes=[s])
            kb.op("act", lambda e: e.activation(out=o[:], in_=pn_[:], func=AF.Copy, scale=s[:, 6:7]), reads=[pn_, s], writes=[o])
            kb.dma("act", O[d, c * 128:(c + 1) * 128, :], o[:], reads=[o], is_output=True)
            kb.op("dve", lambda e: e.tensor_scalar(out=Cf[:], in0=Cf[:], scalar1=s[:, 4:5], scalar2=None, op0=ALU.mult), reads=[Cf, s], writes=[Cf])
            pnn = pn.next()
            for dt_ in range(4):
                pS = pst.next()
                kb.op("pe", lambda e: e.matmul(out=pS[:], lhsT=k_[:, dt_ * 128:(dt_ + 1) * 128], rhs=v_[:], start=True, stop=True), reads=[k_, v_], writes=[pS])
                kb.op("dve", lambda e: e.scalar_tensor_tensor(out=Cf[:, dt_, :], in0=pS[:], scalar=s[:, 4:5], in1=Cf[:, dt_, :], op0=ALU.mult, op1=ALU.add), reads=[pS, s, Cf], writes=[Cf])
                kb.op("pe", lambda e: e.matmul(out=pnn[:, 2 * dt_:2 * dt_ + 1], lhsT=k_[:, dt_ * 128:(dt_ + 1) * 128], rhs=onesb[:], start=True, stop=True), reads=[k_, onesb], writes=[pnn])
            kb.op("act", lambda e: e.activation(out=Cb[:], in_=Cf[:], func=AF.Copy), reads=[Cf], writes=[Cb])
            kb.op("dve", lambda e: e.tensor_scalar(out=nf[:], in0=nf[:], scalar1=s[:, 4:5], scalar2=None, op0=ALU.mult), reads=[nf, s], writes=[nf])
            pnv = pnn[:, 0:8].rearrange("p (a b) -> p a b", b=2)[:, :, 0]
            kb.op("dve", lambda e: e.scalar_tensor_tensor(out=nf[:], in0=pnv, scalar=s[:, 4:5], in1=nf[:], op0=ALU.mult, op1=ALU.add), reads=[pnn, s, nf], writes=[nf])
            kb.op("dve", lambda e: e.tensor_copy(out=nb_[:], in_=nf[:]), reads=[nf], writes=[nb_])
    return kb.finish()

def run_ML(q, k, v, p_lat, p_ctx, inp):
    maps = []
    for c in range(8):
        b, h = c // 4, c % 4
        cols = slice(h * 512, (h + 1) * 512)
        seqs = []
        for d in range(2):
            parts = [s5_seq(a[0][:, :, cols], a[1][:, :, cols], b, d) for a in (q, k, v)]
            gi = 4096 + (2 * d) * 4 + h; gf = 4096 + (2 * d + 1) * 4 + h
            parts.append(s5_seq(p_lat[:, :, gi:gi + 1], p_ctx[:, :, gi:gi + 1], b, d))
            parts.append(s5_seq(p_lat[:, :, gf:gf + 1], p_ctx[:, :, gf:gf + 1], b, d))
            seqs.append(np.concatenate(parts, axis=1))
        QKV = np.ascontiguousarray(np.stack(seqs)).astype(np.float32)
        gbv = inp["ml_gate_b"][0].reshape(4, 4)[:, h]
        GB = np.ascontiguousarray(np.broadcast_to(gbv[None, :], (128, 4))).astype(np.float32)
        jj, ii = np.meshgrid(np.arange(128), np.arange(128), indexing="ij")
        TRI = np.stack([(jj <= ii), np.ones((128, 128), bool)]).astype(np.float32)
        maps.append({"QKV": QKV, "GB": GB, "TRI": TRI, "IDENT": np.eye(128, dtype=np.float32)})
    res = run_bass_kernel_spmd(build_ML(), maps, core_ids=list(range(8)))
    o_lat = np.zeros((2, 2, 8192, 2048), np.float32); o_ctx = np.zeros((2, 2, 256, 2048), np.float32)
    for c in range(8):
        b, h = c // 4, c % 4
        cols = slice(h * 512, (h + 1) * 512)
        Y = res.results[c]["O"]
        for d in range(2):
            cc_, ll = s5_unseq(Y[d], d)
            o_lat[d, b][:, cols] = ll; o_ctx[d, b][:, cols] = cc_
    return o_lat, o_ctx


def build_C1(NT=16):
    D = 1024; W = 2048
    kb = KB()
    HF = kb.dram("HF", [NT * 128, W], F32, "ExternalInput"); HB = kb.dram("HB", [NT * 128, W], F32, "ExternalInput")
    XC = kb.dram("XC", [NT * 128, W], F32, "ExternalInput"); OP = kb.dram("OP", [NT * 128, W], F32, "ExternalInput")
    X = kb.dram("X", [NT * 128, D], F32, "ExternalInput")
    WO = kb.dram("WO", [W, D], F32, "ExternalInput")
    REP = kb.dram("REP", [5, 128, 1024], F32, "ExternalInput")
    IDENT = kb.dram("IDENT", [128, 128], F32, "ExternalInput")
    O = kb.dram("O", [NT * 128, D], F32, "ExternalOutput")
    idf, idb = make_ident(kb, IDENT)
    stg = Ring([kb.sb([128, 1024], F32, name=f"stg{i}") for i in range(2)])
    wo = load_weight_bf16_q(kb, WO, W, D, "wo", stg)
    gn = kb.sb([128, W], name="gn"); sk = kb.sb([128, W], name="sk"); gate = kb.sb([128, D], name="gate")
    kb.dma("sp", gn[:, 0:1024], REP[0, :, :], writes=[gn]); kb.dma("sp", gn[:, 1024:2048], REP[1, :, :], writes=[gn])
    kb.dma("sp", sk[:, 0:1024], REP[2, :, :], writes=[sk]); kb.dma("sp", sk[:, 1024:2048], REP[3, :, :], writes=[sk])
    kb.dma("sp", gate[:], REP[4, :, :], writes=[gate])
    def ring(n, shape, dt=F32, nm="r"):
        return Ring([kb.sb(list(shape), dt, name=f"{nm}{i}") for i in range(n)])
    i_hf = ring(2, [128, W], nm="ihf"); i_hb = ring(2, [128, W], nm="ihb"); i_xc = ring(2, [128, W], nm="ixc"); i_op = ring(2, [128, W], nm="iop")
    i_x = ring(2, [128, D], nm="ix"); ot = ring(2, [128, D], nm="ot")
    rr = kb.sb([128, 4, 512], name="rr"); sq = kb.sb([128, 512], name="sq")
    zb = kb.sb([128, W], BF16, name="zb"); zT = kb.sb([128, 16, 128], BF16, name="zT")
    sm = kb.sb([128, 4], name="sm"); s2 = kb.sb([128, 4], name="s2"); mn = kb.sb([128, 4], name="mn"); vr = kb.sb([128, 4], name="vr"); rsd = kb.sb([128, 4], name="rsd")
    pT = Ring([kb.ps([128, 8, 128], BF16, name=f"pT{i}") for i in range(2)])
    pm = Ring([kb.ps([128, 512], F32, name=f"pm{i}") for i in range(4)])
    for t in range(NT):
        rws = slice(t * 128, (t + 1) * 128)
        hf = i_hf.next(); hb = i_hb.next(); xc = i_xc.next(); op_ = i_op.next(); x = i_x.next(); o = ot.next()
        for (buf, src) in ((hf, HF), (hb, HB), (xc, XC), (op_, OP), (x, X)):
            kb.dma("sp", buf[:], src[rws, :], writes=[buf])
        rr2 = rr[:].rearrange("p h e -> p (h e)")
        kb.op("dve", lambda e: e.tensor_tensor(out=rr2, in0=hf[:], in1=hb[:], op=ALU.add), reads=[hf, hb], writes=[rr])
        kb.op("dve", lambda e: e.tensor_reduce(out=sm[:], in_=rr[:], axis=AX.X, op=ALU.add), reads=[rr], writes=[sm])
        for h in range(4):
            kb.op("act", lambda e: e.activation(out=sq[:], in_=rr[:, h, :], func=AF.Square, accum_out=s2[:, h:h + 1]), reads=[rr], writes=[sq, s2])
        kb.op("dve", lambda e: e.tensor_scalar(out=mn[:], in0=sm[:], scalar1=1.0 / 512, scalar2=None, op0=ALU.mult), reads=[sm], writes=[mn])
        kb.op("dve", lambda e: e.tensor_tensor(out=vr[:], in0=mn[:], in1=mn[:], op=ALU.mult), reads=[mn], writes=[vr])
        kb.op("dve", lambda e: e.scalar_tensor_tensor(out=vr[:], in0=s2[:], scalar=1.0 / 512, in1=vr[:], op0=ALU.mult, op1=ALU.subtract), reads=[s2, vr], writes=[vr])
        kb.op("dve", lambda e: e.tensor_scalar(out=vr[:], in0=vr[:], scalar1=EPS, scalar2=None, op0=ALU.add), reads=[vr], writes=[vr])
        kb.op("act", lambda e: e.activation(out=vr[:], in_=vr[:], func=AF.Sqrt), reads=[vr], writes=[vr])
        kb.op("dve", lambda e: e.reciprocal(out=rsd[:], in_=vr[:]), reads=[vr], writes=[rsd])
        for h in range(4):
            kb.op("dve", lambda e: e.tensor_scalar(out=rr[:, h, :], in0=rr[:, h, :], scalar1=mn[:, h:h + 1], scalar2=rsd[:, h:h + 1], op0=ALU.subtract, op1=ALU.mult),
                  reads=[rr, mn, rsd], writes=[rr])
        kb.op("pool", lambda e: e.tensor_tensor(out=rr2, in0=rr2, in1=gn[:], op=ALU.mult), reads=[rr, gn], writes=[rr])
        kb.op("pool", lambda e: e.tensor_tensor(out=xc[:], in0=xc[:], in1=sk[:], op=ALU.mult), reads=[xc, sk], writes=[xc])
        kb.op("dve", lambda e: e.tensor_tensor(out=rr2, in0=rr2, in1=xc[:], op=ALU.add), reads=[rr, xc], writes=[rr])
        kb.op("act", lambda e: e.activation(out=op_[:], in_=op_[:], func=AF.Sigmoid), reads=[op_], writes=[op_])
        kb.op("dve", lambda e: e.tensor_tensor(out=zb[:], in0=rr2, in1=op_[:], op=ALU.mult), reads=[rr, op_], writes=[zb])
        transpose_tile(kb, zb, zT, idb, pT, W)
        for cb in range(2):
            pp = pm.next(); cs = slice(cb * 512, (cb + 1) * 512)
            for k in range(16):
                kb.op("pe", lambda e: e.matmul(out=pp[:], lhsT=zT[:, k, :], rhs=wo[:, k, cs], start=(k == 0), stop=(k == 15)), reads=[zT, wo], writes=[pp])
            kb.op("dve", lambda e: e.tensor_tensor(out=o[:, cs], in0=pp[:], in1=gate[:, cs], op=ALU.mult), reads=[pp, gate], writes=[o])
            kb.op("pool", lambda e: e.tensor_tensor(out=o[:, cs], in0=o[:, cs], in1=x[:, cs], op=ALU.add), reads=[o, x], writes=[o])
        kb.dma("act", O[rws, :], o[:], reads=[o], is_output=True)
    return kb.finish()

def run_C1(lat, hf_lat, hb_lat, xc_lat, p_lat, inp, m):
    HF = to_tl(hf_lat, None); HB = to_tl(hb_lat, None); XC = to_tl(xc_lat, None); OP = to_tl(p_lat[:, :, 2048:4096], None); X = to_tl(lat, None)
    g = inp["ml_gn_g"][0]; s = inp["ml_skip"][0]
    maps = []
    for c in range(8):
        b = c // 4
        REP = np.stack([rep(g[:1024]), rep(g[1024:]), rep(s[:1024]), rep(s[1024:]), rep(m[b, 1, 2])]).astype(np.float32)
        maps.append({"HF": HF[c], "HB": HB[c], "XC": XC[c], "OP": OP[c], "X": X[c], "WO": inp["ml_w_out"][0], "REP": np.ascontiguousarray(REP),
                     "IDENT": np.eye(128, dtype=np.float32)})
    res = run_bass_kernel_spmd(build_C1(), maps, core_ids=list(range(8)))
    lat2, _ = from_tl([r["O"] for r in res.results], 1024, has_ctx=False)
    return lat2


def build_M():
    kb = KB()
    cT = kb.dram("cT", [128, 8, 3], F32, "ExternalInput")
    W = kb.dram("W", [1024, 1536], F32, "ExternalInput")
    Bv = kb.dram("B", [3, 1536], F32, "ExternalInput")
    O = kb.dram("O", [3, 1536], F32, "ExternalOutput")
    cs = kb.sb([128, 8, 3]); sc = kb.sb([128, 8, 3]); bs = kb.sb([3, 1536]); os_ = kb.sb([3, 1536])
    ws = [kb.sb([128, 1536], name=f"w{k}") for k in range(8)]
    kb.dma("sp", cs[:], cT[:, :, :], writes=[cs])
    kb.dma("sp", bs[:], Bv[:, :], writes=[bs])
    for k in range(8):
        kb.dma("sp" if k % 2 == 0 else "act", ws[k][:], W[k * 128:(k + 1) * 128, :], writes=[ws[k]])
    kb.op("act", lambda e: e.activation(out=sc[:], in_=cs[:], func=AF.Silu), reads=[cs], writes=[sc])
    for j in range(3):
        pm = kb.ps([128, 512], name=f"pm{j}")
        for k in range(8):
            kb.op("pe", lambda e: e.matmul(out=pm[0:3, :], lhsT=sc[:, k, :], rhs=ws[k][:, j * 512:(j + 1) * 512],
                                          start=(k == 0), stop=(k == 7)), reads=[sc, ws[k]], writes=[pm])
        kb.op("dve", lambda e: e.tensor_tensor(out=os_[:, j * 512:(j + 1) * 512], in0=pm[0:3, :],
                                              in1=bs[:, j * 512:(j + 1) * 512], op=ALU.add), reads=[pm, bs], writes=[os_])
    kb.dma("sp", O[:, :], os_[:], reads=[os_], is_output=True)
    return kb.finish()

def run_M(inp):
    cv = np.stack([inp['c'][0], inp['c'][1], inp['c_ctx']], 0)
    cT = np.ascontiguousarray(cv.T.reshape(8, 128, 3).transpose(1, 0, 2))
    Wc = np.concatenate([inp['mod_w'][0], inp['mod_w'][1]], axis=1)
    bc = np.concatenate([inp['mod_b'][0], inp['mod_b'][1]], axis=0)
    maps = []
    for c in range(8):
        sl = slice(c * 1536, (c + 1) * 1536)
        maps.append({"cT": cT, "W": np.ascontiguousarray(Wc[:, sl]),
                     "B": np.ascontiguousarray(np.broadcast_to(bc[sl], (3, 1536)))})
    res = run_bass_kernel_spmd(build_M(), maps, core_ids=list(range(8)))
    m = np.concatenate([r["O"] for r in res.results], axis=1)
    return m.reshape(3, 2, 6, 1024)


def kernel(**inp):
    inp = {k: np.asarray(v) for k, v in inp.items()}
    x, ctx = inp["x"], inp["ctx"]
    m = run_M(inp)
    pl, pc = run_LIN(x, ctx, inp["ev_w_in"][0], inp["norm_mix_g"][0], m, 0, 0, 1)
    s5l, s5c = run_S5(pl[:, :, :512], pc[:, :, :512], inp)
    rl, rc = run_RET(pl, pc, inp)
    lat, cx = run_C0(x, ctx, (s5l[0], s5c[0]), (s5l[1], s5c[1]), (rl[0], rc[0]), (rl[1], rc[1]), pl, pc, inp, m)
    lat, cx = run_MLP(lat, cx, inp, m, 0, False)
    pl, pc = run_LIN(lat, cx, inp["ml_w_in"][0], inp["norm_mix_g"][1], m, 1, 0, 1)
    q, k, v, xc = run_QKV1(pl, pc, inp)
    hl, hc = run_ML(q, k, v, pl, pc, inp)
    lat = run_C1(lat, hl[0], hl[1], xc[0], pl, inp, m)
    out, _ = run_MLP(lat, None, inp, m, 1, True)
    return np.ascontiguousarray(out.astype(np.float32))
```

```python
import numpy as np
from contextlib import ExitStack
import concourse.bass as bass
import concourse.mybir as mybir
from concourse.bass_utils import run_bass_kernel_spmd

F32 = mybir.dt.float32
BF16 = mybir.dt.bfloat16
AF = mybir.ActivationFunctionType
ALU = mybir.AluOpType
AX = mybir.AxisListType


class Buf:
    def __init__(self, t, name):
        self.t = t
        self.name = name
        self.lw = None
        self.rd = []
        self.dsem = None
        self.dval = 0

    def __getitem__(self, idx):
        return self.t[idx]


class KB:
    ENG = ("pe", "dve", "act", "pool", "sp")

    def __init__(self, same_engine_sync=True):
        self.nc = bass.Bass("TRN2", target_bir_lowering=False)
        nc = self.nc
        self.es = ExitStack()
        self.e = dict(pe=nc.tensor, dve=nc.vector, act=nc.scalar, pool=nc.gpsimd, sp=nc.sync)
        self.sem = {k: self.es.enter_context(nc.semaphore("sem_" + k)) for k in self.ENG}
        self.cnt = {k: 0 for k in self.ENG}
        self.seen = {k: {} for k in self.ENG}
        self.same = same_engine_sync
        self.nbuf = 0
        self.out_recs = []
        self.ninstr = 0

    def dram(self, name, shape, dtype, kind):
        return self.nc.dram_tensor(name, list(shape), dtype, kind=kind).ap()

    def sb(self, shape, dtype=F32, name=None):
        self.nbuf += 1
        name = name or f"sb{self.nbuf}"
        t = self.es.enter_context(self.nc.sbuf_tensor(name, list(shape), dtype))
        return Buf(t, name)

    def ps(self, shape, dtype=F32, name=None):
        self.nbuf += 1
        name = name or f"ps{self.nbuf}"
        t = self.es.enter_context(self.nc.psum_tensor(name, list(shape), dtype))
        return Buf(t, name)

    def _wait(self, e, rec):
        if rec is None:
            return
        kind, key, val = rec
        if kind == "eng":
            if key == e and (not self.same or e in ("pe",)):
                return
            sem = self.sem[key]
        else:
            sem = key
        sid = id(sem)
        if self.seen[e].get(sid, 0) >= val:
            return
        self.seen[e][sid] = val
        self.e[e].wait_ge(sem, val)
        self.ninstr += 1

    def _deps(self, e, reads, writes):
        for b in reads:
            self._wait(e, b.lw)
        for b in writes:
            self._wait(e, b.lw)
            for r in b.rd:
                if r[0] == "eng" and r[1] == e and e != "pool":
                    continue
                self._wait(e, r)

    def _record(self, rec, reads, writes):
        for b in reads:
            b.rd.append(rec)
        for b in writes:
            b.lw = rec
            b.rd = []

    def op(self, e, fn, reads=(), writes=()):
        reads = [b for b in reads if isinstance(b, Buf)]
        writes = [b for b in writes if isinstance(b, Buf)]
        self._deps(e, reads, writes)
        ins = fn(self.e[e])
        self.cnt[e] += 1
        ins.then_inc(self.sem[e], 1)
        self.ninstr += 1
        self._record(("eng", e, self.cnt[e]), reads, writes)
        return ins

    def dma(self, q, out, in_, reads=(), writes=(), is_output=False):
        reads = [b for b in reads if isinstance(b, Buf)]
        writes = [b for b in writes if isinstance(b, Buf)]
        self._deps(q, reads, writes)
        owner = writes[0] if writes else reads[0]
        if owner.dsem is None:
            owner.dsem = self.es.enter_context(self.nc.semaphore("dsem_" + owner.name))
        owner.dval += 16
        self.e[q].dma_start(out=out, in_=in_).then_inc(owner.dsem, 16)
        self.ninstr += 1
        rec = ("dma", owner.dsem, owner.dval)
        self._record(rec, reads, writes)
        if is_output:
            self.out_recs.append(rec)
        return rec

    def finish(self):
        for rec in self.out_recs:
            self._wait("sp", rec)
        for k in ("pe", "dve", "act", "pool"):
            if self.cnt[k]:
                self._wait("sp", ("eng", k, self.cnt[k]))
        self.es.close()
        return self.nc


EPS = 1e-6

class Ring:
    def __init__(self, bufs):
        self.b = bufs; self.i = 0
    def next(self):
        b = self.b[self.i % len(self.b)]; self.i += 1
        return b

def load_weight_bf16(kb, Wd, K, N, name, queues=("sp", "pool")):
    KC = K // 128
    wb = kb.sb([128, KC, N], BF16, name=name)
    stg = Ring([kb.sb([128, N], F32, name=f"{name}_stg{i}") for i in range(2)])
    for k in range(KC):
        s = stg.next()
        kb.dma(queues[k % len(queues)], s[:], Wd[k * 128:(k + 1) * 128, :], writes=[s])
        if k % 2 == 0:
            kb.op("dve", lambda e: e.tensor_copy(out=wb[:, k, :], in_=s[:]), reads=[s], writes=[wb])
        else:
            kb.op("pool", lambda e: e.tensor_copy(out=wb[:, k, :], in_=s[:]), reads=[s], writes=[wb])
    return wb

def rms_rstd(kb, xt, scr, ss, rstd, D):
    kb.op("act", lambda e: e.activation(out=scr[:], in_=xt[:], func=AF.Square, accum_out=ss[:]), reads=[xt], writes=[scr, ss])
    kb.op("dve", lambda e: e.tensor_scalar(out=ss[:], in0=ss[:], scalar1=1.0 / D, scalar2=EPS, op0=ALU.mult, op1=ALU.add), reads=[ss], writes=[ss])
    kb.op("act", lambda e: e.activation(out=ss[:], in_=ss[:], func=AF.Sqrt), reads=[ss], writes=[ss])
    kb.op("dve", lambda e: e.reciprocal(out=rstd[:], in_=ss[:]), reads=[ss], writes=[rstd])

def transpose_tile(kb, src, dst, ident, pT, ncol, evac="act"):
    nk = ncol // 128
    for k0 in range(0, nk, 8):
        kk = min(8, nk - k0)
        p = pT.next()
        for k in range(kk):
            kb.op("pe", lambda e: e.transpose(out=p[:, k, :], in_=src[:, (k0 + k) * 128:(k0 + k + 1) * 128], identity=ident[:]),
                  reads=[src, ident], writes=[p])
        if evac == "act":
            kb.op("act", lambda e: e.activation(out=dst[:, k0:k0 + kk, :], in_=p[:, 0:kk, :], func=AF.Copy), reads=[p], writes=[dst])
        else:
            kb.op("dve", lambda e: e.tensor_copy(out=dst[:, k0:k0 + kk, :], in_=p[:, 0:kk, :]), reads=[p], writes=[dst])

def make_ident(kb, identd):
    idf = kb.sb([128, 128], F32, name="idf"); idb = kb.sb([128, 128], BF16, name="idb")
    kb.dma("sp", idf[:], identd[:, :], writes=[idf])
    kb.op("dve", lambda e: e.tensor_copy(out=idb[:], in_=idf[:]), reads=[idf], writes=[idb])
    return idf, idb

def build_LIN(NT, N, n_lat_tiles=16):
    D = 1024
    kb = KB()
    X = kb.dram("X", [NT * 128, D], F32, "ExternalInput")
    W = kb.dram("W", [D, N], F32, "ExternalInput")
    MOD = kb.dram("MOD", [2, 3, 128, D], F32, "ExternalInput")
    IDENT = kb.dram("IDENT", [128, 128], F32, "ExternalInput")
    O = kb.dram("O", [NT * 128, N], F32, "ExternalOutput")
    idf, idb = make_ident(kb, IDENT)
    wb = load_weight_bf16(kb, W, D, N, "wb")
    G1 = []; SH = []
    for r in range(2):
        g = kb.sb([128, D], name=f"g{r}"); sh = kb.sb([128, D], name=f"sh{r}"); sc = kb.sb([128, D], name=f"sc{r}")
        kb.dma("sp", g[:], MOD[r, 0, :, :], writes=[g]); kb.dma("sp", sh[:], MOD[r, 1, :, :], writes=[sh])
        kb.dma("sp", sc[:], MOD[r, 2, :, :], writes=[sc])
        kb.op("dve", lambda e: e.scalar_tensor_tensor(out=g[:], in0=sc[:], scalar=1.0, in1=g[:], op0=ALU.add, op1=ALU.mult), reads=[sc, g], writes=[g])
        G1.append(g); SH.append(sh)
    xr = Ring([kb.sb([128, D], name=f"x{i}") for i in range(2)])
    scr = kb.sb([128, D], name="scr")
    tmp = Ring([kb.sb([128, D], name=f"tmp{i}") for i in range(2)])
    hb = Ring([kb.sb([128, D], BF16, name=f"hb{i}") for i in range(2)])
    hT = Ring([kb.sb([128, 8, 128], BF16, name=f"hT{i}") for i in range(2)])
    ss = Ring([kb.sb([128, 1], name=f"ss{i}") for i in range(2)]); rs = Ring([kb.sb([128, 1], name=f"rs{i}") for i in range(2)])
    pT = Ring([kb.ps([128, 8, 128], BF16, name=f"pT{i}") for i in range(2)])
    pm = Ring([kb.ps([128, 512], F32, name=f"pm{i}") for i in range(4)])
    ot = Ring([kb.sb([128, N], name=f"ot{i}") for i in range(2)])
    blocks = [(s, min(512, N - s)) for s in range(0, N, 512)]
    ev = 0
    for t in range(NT):
        r = 0 if t < n_lat_tiles else 1
        x = xr.next(); s_ = ss.next(); rstd = rs.next(); tm = tmp.next(); h = hb.next(); hTt = hT.next(); o = ot.next()
        kb.dma("sp", x[:], X[t * 128:(t + 1) * 128, :], writes=[x])
        rms_rstd(kb, x, scr, s_, rstd, D)
        kb.op("dve", lambda e: e.scalar_tensor_tensor(out=tm[:], in0=x[:], scalar=rstd[:], in1=G1[r][:], op0=ALU.mult, op1=ALU.mult), reads=[x, rstd, G1[r]], writes=[tm])
        kb.op("pool", lambda e: e.tensor_tensor(out=h[:], in0=tm[:], in1=SH[r][:], op=ALU.add), reads=[tm, SH[r]], writes=[h])
        transpose_tile(kb, h, hTt, idb, pT, D)
        for (s0, w) in blocks:
            p = pm.next()
            for k in range(8):
                kb.op("pe", lambda e: e.matmul(out=p[:, 0:w], lhsT=hTt[:, k, :], rhs=wb[:, k, s0:s0 + w], start=(k == 0), stop=(k == 7)),
                      reads=[hTt, wb], writes=[p])
            if ev % 2 == 0:
                kb.op("act", lambda e: e.activation(out=o[:, s0:s0 + w], in_=p[:, 0:w], func=AF.Copy), reads=[p], writes=[o])
            else:
                kb.op("dve", lambda e: e.tensor_copy(out=o[:, s0:s0 + w], in_=p[:, 0:w]), reads=[p], writes=[o])
            ev += 1
        kb.dma("pool", O[t * 128:(t + 1) * 128, :], o[:], reads=[o], is_output=True)
    return kb.finish()

def to_tl(lat, ctx):
    out = []
    for c in range(8):
        b, q = c // 4, c % 4
        parts = [lat[b, q * 2048:(q + 1) * 2048]]
        if ctx is not None:
            pad = np.zeros((128, lat.shape[-1]), lat.dtype)
            pad[:64] = ctx[b, q * 64:(q + 1) * 64]
            parts.append(pad)
        out.append(np.ascontiguousarray(np.concatenate(parts, 0)))
    return out

def from_tl(outs, W, has_ctx=True):
    lat = np.empty((2, 8192, W), np.float32); ctx = np.empty((2, 256, W), np.float32) if has_ctx else None
    for c in range(8):
        b, q = c // 4, c % 4
        lat[b, q * 2048:(q + 1) * 2048] = outs[c][:2048]
        if has_ctx:
            ctx[b, q * 64:(q + 1) * 64] = outs[c][2048:2048 + 64]
    return lat, ctx

def rep(v):
    return np.broadcast_to(np.asarray(v, np.float32)[None, :], (128, v.shape[-1]))

def run_LIN(lat, ctx, W, g, m, layer, j_shift, j_scale):
    N = W.shape[1]
    X = to_tl(lat, ctx)
    maps = []
    for c in range(8):
        b = c // 4
        MOD = np.stack([np.stack([rep(g), rep(m[b, layer, j_shift]), rep(m[b, layer, j_scale])]),
                        np.stack([rep(g), rep(m[2, layer, j_shift]), rep(m[2, layer, j_scale])])]).astype(np.float32)
        maps.append({"X": X[c], "W": np.ascontiguousarray(W), "MOD": np.ascontiguousarray(MOD), "IDENT": np.eye(128, dtype=np.float32)})
    res = run_bass_kernel_spmd(build_LIN(17, N), maps, core_ids=list(range(8)))
    return from_tl([r["O"] for r in res.results], N)


def load_weight_bf16_q(kb, Wd, K, N, name, stg, cw=1024):
    KC = K // 128
    wb = kb.sb([128, KC, N], BF16, name=name)
    i = 0
    for k in range(KC):
        for c0 in range(0, N, cw):
            s = stg.next()
            kb.dma("sp" if i % 2 == 0 else "pool", s[:, 0:cw], Wd[k * 128:(k + 1) * 128, c0:c0 + cw], writes=[s])
            eng = "dve" if i % 2 == 0 else "pool"
            kb.op(eng, lambda e: e.tensor_copy(out=wb[:, k, c0:c0 + cw], in_=s[:, 0:cw]), reads=[s], writes=[wb])
            i += 1
    return wb

def build_MLP(NT, final, n_lat_tiles=16):
    D = 1024; H = 4096
    kb = KB()
    X = kb.dram("X", [NT * 128, D], F32, "ExternalInput")
    W1 = kb.dram("W1", [D, H], F32, "ExternalInput")
    W2 = kb.dram("W2", [H, D], F32, "ExternalInput")
    MODF = kb.dram("MODF", [2, 128, 3, 8], F32, "ExternalInput")
    GATE = kb.dram("GATE", [2, 128, D], F32, "ExternalInput")
    FG = kb.dram("FG", [128, D], F32, "ExternalInput")
    IDENT = kb.dram("IDENT", [128, 128], F32, "ExternalInput")
    O = kb.dram("O", [NT * 128, D], F32, "ExternalOutput")
    idf, idb = make_ident(kb, IDENT)
    stg = Ring([kb.sb([128, 1024], F32, name=f"stg{i}") for i in range(2)])
    w1b = load_weight_bf16_q(kb, W1, D, H, "w1b", stg)
    w2b = load_weight_bf16_q(kb, W2, H, D, "w2b", stg)
    G1 = []; SH = []; GT = []
    nvar = 2 if NT > n_lat_tiles else 1
    for r in range(nvar):
        mf = kb.sb([128, 3, 8], name=f"mf{r}"); g1 = kb.sb([128, 8], name=f"g1{r}"); gt = kb.sb([128, D], name=f"gt{r}")
        kb.dma("sp", mf[:], MODF[r, :, :, :], writes=[mf]); kb.dma("sp", gt[:], GATE[r, :, :], writes=[gt])
        kb.op("dve", lambda e: e.scalar_tensor_tensor(out=g1[:], in0=mf[:, 2, :], scalar=1.0, in1=mf[:, 0, :], op0=ALU.add, op1=ALU.mult), reads=[mf], writes=[g1])
        G1.append(g1); SH.append(mf); GT.append(gt)
    if final:
        fg = kb.sb([128, D], name="fg"); kb.dma("sp", fg[:], FG[:, :], writes=[fg])
    xr = Ring([kb.sb([128, D], name=f"x{i}") for i in range(2)])
    ot = Ring([kb.sb([128, D], name=f"ot{i}") for i in range(2)])
    xnr = Ring([kb.sb([128, D], BF16, name=f"xn{i}") for i in range(2)])
    hTr = Ring([kb.sb([128, 8, 128], BF16, name=f"hT{i}") for i in range(2)])
    hidr = Ring([kb.sb([128, 32, 128], BF16, name=f"hid{i}") for i in range(2)])
    rl = Ring([kb.sb([128, 512], name=f"rl{i}") for i in range(2)])
    ss = Ring([kb.sb([128, 1], name=f"ss{i}") for i in range(2)]); rs = Ring([kb.sb([128, 1], name=f"rs{i}") for i in range(2)])
    pT = Ring([kb.ps([128, 8, 128], BF16, name=f"pT{i}") for i in range(2)])
    pm = Ring([kb.ps([128, 512], F32, name=f"pm{i}") for i in range(5)])
    for t in range(NT):
        r = 0 if t < n_lat_tiles else 1
        x = xr.next(); s_ = ss.next(); rstd = rs.next(); o = ot.next(); xn = xnr.next(); hT = hTr.next(); hid = hidr.next()
        kb.dma("sp", x[:], X[t * 128:(t + 1) * 128, :], writes=[x])
        rms_rstd(kb, x, o, s_, rstd, D)
        kb.op("dve", lambda e: e.tensor_scalar(out=xn[:], in0=x[:], scalar1=rstd[:], scalar2=None, op0=ALU.mult), reads=[x, rstd], writes=[xn])
        p = pT.next()
        for k in range(8):
            kb.op("pe", lambda e: e.transpose(out=p[:, k, :], in_=xn[:, k * 128:(k + 1) * 128], identity=idb[:]), reads=[xn, idb], writes=[p])
        for k in range(8):
            kb.op("act", lambda e: e.activation(out=hT[:, k, :], in_=p[:, k, :], func=AF.Identity, scale=G1[r][:, k:k + 1], bias=SH[r][:, 1, k:k + 1]),
                  reads=[p, G1[r], SH[r]], writes=[hT])
        for f4 in range(8):
            pp = pm.next()
            for fi in range(4):
                f = f4 * 4 + fi
                for k in range(8):
                    kb.op("pe", lambda e: e.matmul(out=pp[:, fi * 128:(fi + 1) * 128], lhsT=w1b[:, k, f * 128:(f + 1) * 128], rhs=hT[:, k, :],
                                                  start=(k == 0), stop=(k == 7)), reads=[w1b, hT], writes=[pp])
            rr = rl.next()
            kb.op("act", lambda e: e.activation(out=rr[:], in_=pp[:], func=AF.Relu), reads=[pp], writes=[rr])
            kb.op("pool" if f4 % 2 else "dve", lambda e: e.tensor_tensor(out=hid[:, f4 * 4:(f4 + 1) * 4, :], in0=rr[:], in1=rr[:], op=ALU.mult), reads=[rr], writes=[hid])
        for cb in range(2):
            pp = pm.next()
            for f in range(32):
                kb.op("pe", lambda e: e.matmul(out=pp[:], lhsT=hid[:, f, :], rhs=w2b[:, f, cb * 512:(cb + 1) * 512], start=(f == 0), stop=(f == 31)),
                      reads=[hid, w2b], writes=[pp])
            cs = slice(cb * 512, (cb + 1) * 512)
            kb.op("dve", lambda e: e.tensor_tensor(out=o[:, cs], in0=pp[:], in1=GT[r][:, cs], op=ALU.mult), reads=[pp, GT[r]], writes=[o])
            kb.op("pool", lambda e: e.tensor_tensor(out=o[:, cs], in0=o[:, cs], in1=x[:, cs], op=ALU.add), reads=[o, x], writes=[o])
        if final:
            rms_rstd(kb, o, x, s_, rstd, D)
            kb.op("dve", lambda e: e.scalar_tensor_tensor(out=o[:], in0=o[:], scalar=rstd[:], in1=fg[:], op0=ALU.mult, op1=ALU.mult), reads=[o, rstd, fg], writes=[o])
        kb.dma("pool", O[t * 128:(t + 1) * 128, :], o[:], reads=[o], is_output=True)
    return kb.finish()

def featmaj(v):
    return np.asarray(v, np.float32).reshape(8, 128).T

def run_MLP(lat, ctx, inp, m, layer, final):
    X = to_tl(lat, ctx)
    NT = 17 if ctx is not None else 16
    g = inp["norm_mlp_g"][layer]
    maps = []
    for c in range(8):
        b = c // 4
        MODF = np.stack([np.stack([featmaj(g), featmaj(m[r, layer, 3]), featmaj(m[r, layer, 4])], 1) for r in (b, 2)]).astype(np.float32)
        GATE = np.stack([rep(m[b, layer, 5]), rep(m[2, layer, 5])]).astype(np.float32)
        maps.append({"X": X[c], "W1": inp["mlp_w1"][layer], "W2": inp["mlp_w2"][layer], "MODF": np.ascontiguousarray(MODF),
                     "GATE": np.ascontiguousarray(GATE), "FG": np.ascontiguousarray(rep(inp["final_norm_g"])), "IDENT": np.eye(128, dtype=np.float32)})
    res = run_bass_kernel_spmd(build_MLP(NT, final), maps, core_ids=list(range(8)))
    return from_tl([r["O"] for r in res.results], 1024, has_ctx=ctx is not None)


import math

L_S5 = 8448
BLK = 512
PAD = 256

def build_S5():
    kb = KB()
    U = kb.dram("U", [2, 128, L_S5], F32, "ExternalInput")
    PRM = kb.dram("PRM", [128, 3, 8], F32, "ExternalInput")
    BB = kb.dram("BB", [128, 2, 8, 16], F32, "ExternalInput")
    CC = kb.dram("CC", [128, 2, 8, 16], F32, "ExternalInput")
    IDENT = kb.dram("IDENT", [128, 128], F32, "ExternalInput")
    Y = kb.dram("Y", [2, 128, L_S5], F32, "ExternalOutput")
    idf, idb = make_ident(kb, IDENT)
    prm = kb.sb([128, 3, 8], name="prm"); bb = kb.sb([128, 2, 8, 16], name="bb"); cc = kb.sb([128, 2, 8, 16], name="cc")
    kb.dma("sp", prm[:], PRM[:, :, :], writes=[prm]); kb.dma("sp", bb[:], BB[:, :, :, :], writes=[bb]); kb.dma("sp", cc[:], CC[:, :, :, :], writes=[cc])
    ub = [kb.sb([128, L_S5], BF16, name=f"ub{d}") for d in range(2)]
    ust = Ring([kb.sb([128, 2112], name=f"ust{i}") for i in range(2)])
    for d in range(2):
        for sgi in range(4):
            s = ust.next()
            kb.dma("sp" if sgi % 2 == 0 else "pool", s[:], U[d, :, sgi * 2112:(sgi + 1) * 2112], writes=[s])
            kb.op("act", lambda e: e.activation(out=ub[d][:, sgi * 2112:(sgi + 1) * 2112], in_=s[:], func=AF.Copy), reads=[s], writes=[ub[d]])
    def T(name, shape=(128, 8)):
        return kb.sb(list(shape), name=name)
    def tt(out, a, b, op, eng="dve"):
        kb.op(eng, lambda e: e.tensor_tensor(out=out, in0=a, in1=b, op=op), reads=[prmbuf], writes=[prmbuf])
    prmbuf = Buf(None, "prmbuf")
    def vv(out_t, a, b, op):
        kb.op("dve", lambda e: e.tensor_tensor(out=out_t[:], in0=a, in1=b, op=op), reads=[prmbuf, prm, bb, cc], writes=[prmbuf])
    def act(out_t, a, func, **kw):
        kb.op("act", lambda e: e.activation(out=out_t[:], in_=a, func=func, **kw), reads=[prmbuf, prm], writes=[prmbuf])
    def ts(out_t, a, s1, s2, op0, op1=None):
        if op1 is None:
            kb.op("dve", lambda e: e.tensor_scalar(out=out_t[:], in0=a, scalar1=s1, scalar2=None, op0=op0), reads=[prmbuf, prm], writes=[prmbuf])
        else:
            kb.op("dve", lambda e: e.tensor_scalar(out=out_t[:], in0=a, scalar1=s1, scalar2=s2, op0=op0, op1=op1), reads=[prmbuf, prm], writes=[prmbuf])
    lre = prm[:, 0, :]; lim = prm[:, 1, :]; lst = prm[:, 2, :]
    step = T("step"); a_ = T("a_"); th = T("th"); ea = T("ea"); c_ = T("c_"); s_ = T("s_"); t1 = T("t1"); t2 = T("t2"); t3 = T("t3")
    hpi = T("hpi", (128, 1))
    kb.op("dve", lambda e: e.memset(hpi[:], math.pi / 2), reads=[prmbuf], writes=[prmbuf])
    act(step, lst, AF.Exp)
    vv(a_, lre, step[:], ALU.mult); vv(th, lim, step[:], ALU.mult)
    act(ea, a_[:], AF.Exp)
    act(s_, th[:], AF.Sin, scale=1.0 / 16)
    act(c_, th[:], AF.Sin, scale=1.0 / 16, bias=hpi[:])
    for _ in range(4):
        vv(t1, c_[:], c_[:], ALU.mult); vv(t2, s_[:], s_[:], ALU.mult); vv(t3, c_[:], s_[:], ALU.mult)
        vv(c_, t1[:], t2[:], ALU.subtract); ts(s_, t3[:], 2.0, None, ALU.mult)
    NLEV = 9
    PR = [T(f"pr{k}") for k in range(NLEV)]; PI = [T(f"pi{k}") for k in range(NLEV)]; NPI = [T(f"npi{k}") for k in range(NLEV)]
    vv(PR[0], ea[:], c_[:], ALU.mult); vv(PI[0], ea[:], s_[:], ALU.mult)
    for k in range(1, NLEV):
        vv(t1, PR[k - 1][:], PR[k - 1][:], ALU.mult); vv(t2, PI[k - 1][:], PI[k - 1][:], ALU.mult); vv(t3, PR[k - 1][:], PI[k - 1][:], ALU.mult)
        vv(PR[k], t1[:], t2[:], ALU.subtract); ts(PI[k], t3[:], 2.0, None, ALU.mult)
    for k in range(NLEV):
        ts(NPI[k], PI[k][:], -1.0, None, ALU.mult)
    lm1 = T("lm1"); nr = T("nr"); ni = T("ni"); den = T("den"); cr = T("cr"); ci = T("ci"); nci = T("nci")
    ts(lm1, PR[0][:], -1.0, None, ALU.add)
    vv(t1, lm1[:], lre, ALU.mult); vv(t2, PI[0][:], lim, ALU.mult); vv(nr, t1[:], t2[:], ALU.add)
    vv(t1, PI[0][:], lre, ALU.mult); vv(t2, lm1[:], lim, ALU.mult); vv(ni, t1[:], t2[:], ALU.subtract)
    vv(t1, lre, lre, ALU.mult); vv(t2, lim, lim, ALU.mult); vv(den, t1[:], t2[:], ALU.add)
    kb.op("dve", lambda e: e.reciprocal(out=den[:], in_=den[:]), reads=[prmbuf], writes=[prmbuf])
    vv(cr, nr[:], den[:], ALU.mult); vv(ci, ni[:], den[:], ALU.mult); ts(nci, ci[:], -1.0, None, ALU.mult)
    Bl = [[kb.sb([128, 128], BF16, name=f"Bl{j}_{c}") for c in range(2)] for j in range(8)]
    Cl = [[kb.sb([128, 128], BF16, name=f"Cl{j}_{c}") for c in range(2)] for j in range(8)]
    bsrc = kb.sb([128, 128], name="bsrc"); bt = T("bt", (128, 16)); bt2 = T("bt2", (128, 16))
    pTf = kb.ps([128, 128], F32, name="pTf")
    for j in range(8):
        q = j % 4
        for c in range(2):
            x1 = bb[:, 0, j, :] if c == 0 else bb[:, 1, j, :]
            x2 = bb[:, 1, j, :] if c == 0 else bb[:, 0, j, :]
            sc2 = nci if c == 0 else ci
            kb.op("dve", lambda e: e.tensor_scalar(out=bt[:], in0=x1, scalar1=cr[:, j:j + 1], scalar2=None, op0=ALU.mult), reads=[prmbuf, bb], writes=[prmbuf])
            kb.op("dve", lambda e: e.scalar_tensor_tensor(out=bt2[:], in0=x2, scalar=sc2[:, j:j + 1], in1=bt[:], op0=ALU.mult, op1=ALU.add), reads=[prmbuf, bb], writes=[prmbuf])
            kb.op("dve", lambda e: e.memset(bsrc[:], 0.0), reads=[prmbuf, bsrc], writes=[prmbuf, bsrc])
            kb.op("dve", lambda e: e.tensor_copy(out=bsrc[0:64, 32 * q:32 * q + 16], in_=bt2[0:64, :]), reads=[prmbuf, bsrc], writes=[prmbuf, bsrc])
            kb.op("dve", lambda e: e.tensor_copy(out=bsrc[64:128, 32 * q + 16:32 * q + 32], in_=bt2[64:128, :]), reads=[prmbuf, bsrc], writes=[prmbuf, bsrc])
            kb.op("pe", lambda e: e.transpose(out=pTf[:], in_=bsrc[:], identity=idf[:]), reads=[bsrc, idf], writes=[pTf])
            kb.op("act", lambda e: e.activation(out=Bl[j][c][:], in_=pTf[:], func=AF.Copy), reads=[pTf], writes=[Bl[j][c]])
            kb.op("pool", lambda e: e.memset(Cl[j][c][:], 0.0), writes=[Cl[j][c]])
            sgn = 1.0 if c == 0 else -1.0
            kb.op("act", lambda e: e.activation(out=Cl[j][c][0:64, 32 * q:32 * q + 16], in_=cc[0:64, c, j, :], func=AF.Copy, scale=sgn), reads=[cc], writes=[Cl[j][c]])
            kb.op("act", lambda e: e.activation(out=Cl[j][c][64:128, 32 * q + 16:32 * q + 32], in_=cc[64:128, c, j, :], func=AF.Copy, scale=sgn), reads=[cc], writes=[Cl[j][c]])
    W = PAD + BLK
    sets = {}
    for d in range(2):
        sets[d] = []
        for i in range(2):
            A = kb.sb([128, 2, W], name=f"A{d}{i}"); B = kb.sb([128, 2, W], name=f"B{d}{i}"); Tt = kb.sb([128, 2, BLK], name=f"T{d}{i}")
            kb.op("pool", lambda e: e.memset(A[:], 0.0), writes=[A]); kb.op("pool", lambda e: e.memset(B[:], 0.0), writes=[B])
            sets[d].append((A, B, Tt))
    hb = Ring([kb.sb([128, 2, BLK], BF16, name=f"hb{i}") for i in range(4)])
    px = Ring([kb.ps([128, 512], name=f"px{i}") for i in range(4)])
    py = Ring([kb.ps([128, 512], name=f"py{i}") for i in range(2)])
    ytr = Ring([kb.sb([128, 512], name=f"yt{i}") for i in range(4)])
    blocks = [(s0, min(BLK, L_S5 - s0)) for s0 in range(0, L_S5, BLK)]
    cnt = {0: 0, 1: 0}
    for q in range(4):
        prevs = {0: None, 1: None}
        for (s0, w) in blocks:
            st = {}
            for d in range(2):
                j = d * 4 + q
                prev = prevs[d]
                A, B, Tt = sets[d][cnt[d] % 2]; cnt[d] += 1
                pr_ = px.next(); pi_ = px.next()
                kb.op("pe", lambda e: e.matmul(out=pr_[:, 0:w], lhsT=Bl[j][0][:], rhs=ub[d][:, s0:s0 + w], start=True, stop=True), reads=[Bl[j][0], ub[d]], writes=[pr_])
                kb.op("pe", lambda e: e.matmul(out=pi_[:, 0:w], lhsT=Bl[j][1][:], rhs=ub[d][:, s0:s0 + w], start=True, stop=True), reads=[Bl[j][1], ub[d]], writes=[pi_])
                kb.op("act", lambda e: e.activation(out=A[:, 0, PAD:PAD + w], in_=pr_[:, 0:w], func=AF.Copy), reads=[pr_], writes=[A])
                kb.op("act", lambda e: e.activation(out=A[:, 1, PAD:PAD + w], in_=pi_[:, 0:w], func=AF.Copy), reads=[pi_], writes=[A])
                if prev is not None:
                    pA, pw = prev
                    cre_ = pA[:, 0, PAD + pw - 1:PAD + pw]; cim_ = pA[:, 1, PAD + pw - 1:PAD + pw]
                    a0r = A[:, 0, PAD:PAD + 1]; a0i = A[:, 1, PAD:PAD + 1]
                    kb.op("dve", lambda e: e.scalar_tensor_tensor(out=a0r, in0=cre_, scalar=PR[0][:, j:j + 1], in1=a0r, op0=ALU.mult, op1=ALU.add), reads=[pA, A, prmbuf], writes=[A])
                    kb.op("dve", lambda e: e.scalar_tensor_tensor(out=a0r, in0=cim_, scalar=NPI[0][:, j:j + 1], in1=a0r, op0=ALU.mult, op1=ALU.add), reads=[pA, A, prmbuf], writes=[A])
                    kb.op("dve", lambda e: e.scalar_tensor_tensor(out=a0i, in0=cim_, scalar=PR[0][:, j:j + 1], in1=a0i, op0=ALU.mult, op1=ALU.add), reads=[pA, A, prmbuf], writes=[A])
                    kb.op("dve", lambda e: e.scalar_tensor_tensor(out=a0i, in0=cre_, scalar=PI[0][:, j:j + 1], in1=a0i, op0=ALU.mult, op1=ALU.add), reads=[pA, A, prmbuf], writes=[A])
                st[d] = [A, B, Tt]
            nlev = int(math.ceil(math.log2(w)))
            for k in range(nlev):
                sh = 1 << k
                for d in range(2):
                    j = d * 4 + q
                    src, dst, Tt = st[d]
                    s_sh = src[:, :, PAD - sh:PAD - sh + w]
                    kb.op("dve", lambda e: e.scalar_tensor_tensor(out=Tt[:, :, 0:w], in0=s_sh, scalar=PR[k][:, j:j + 1], in1=src[:, :, PAD:PAD + w], op0=ALU.mult, op1=ALU.add),
                          reads=[src, prmbuf], writes=[Tt])
                for d in range(2):
                    j = d * 4 + q
                    src, dst, Tt = st[d]
                    kb.op("dve", lambda e: e.scalar_tensor_tensor(out=dst[:, 0, PAD:PAD + w], in0=src[:, 1, PAD - sh:PAD - sh + w], scalar=NPI[k][:, j:j + 1], in1=Tt[:, 0, 0:w], op0=ALU.mult, op1=ALU.add),
                          reads=[src, Tt, prmbuf], writes=[dst])
                for d in range(2):
                    j = d * 4 + q
                    src, dst, Tt = st[d]
                    kb.op("dve", lambda e: e.scalar_tensor_tensor(out=dst[:, 1, PAD:PAD + w], in0=src[:, 0, PAD - sh:PAD - sh + w], scalar=PI[k][:, j:j + 1], in1=Tt[:, 1, 0:w], op0=ALU.mult, op1=ALU.add),
                          reads=[src, Tt, prmbuf], writes=[dst])
                    st[d] = [dst, src, Tt]
            for d in range(2):
                j = d * 4 + q
                fin = st[d][0]
                h = hb.next()
                kb.op("act", lambda e: e.activation(out=h[:, :, 0:w], in_=fin[:, :, PAD:PAD + w], func=AF.Copy), reads=[fin], writes=[h])
                p_y = py.next()
                kb.op("pe", lambda e: e.matmul(out=p_y[:, 0:w], lhsT=Cl[j][0][:], rhs=h[:, 0, 0:w], start=True, stop=False), reads=[Cl[j][0], h], writes=[p_y])
                kb.op("pe", lambda e: e.matmul(out=p_y[:, 0:w], lhsT=Cl[j][1][:], rhs=h[:, 1, 0:w], start=False, stop=True), reads=[Cl[j][1], h], writes=[p_y])
                yt = ytr.next()
                kb.op("act", lambda e: e.activation(out=yt[32 * q:32 * q + 32, 0:w], in_=p_y[32 * q:32 * q + 32, 0:w], func=AF.Copy), reads=[p_y], writes=[yt])
                kb.dma("sp", Y[d, 32 * q:32 * q + 32, s0:s0 + w], yt[32 * q:32 * q + 32, 0:w], reads=[yt], is_output=True)
                prevs[d] = (fin, w)
    return kb.finish()

def s5_seq(lat, ctx, b, d):
    if d == 0:
        return np.concatenate([ctx[b], lat[b]], 0)
    return np.concatenate([ctx[b][::-1], lat[b][::-1]], 0)

def s5_unseq(y, d):
    c, l = y[:256], y[256:]
    if d == 1:
        c, l = c[::-1], l[::-1]
    return c, l

def run_S5(u_lat, u_ctx, inp):
    i = 0
    maps = []
    for c in range(8):
        b, gb = c // 4, c % 4
        cols = slice(gb * 128, (gb + 1) * 128)
        U = np.stack([np.ascontiguousarray(s5_seq(u_lat, u_ctx, b, d)[:, cols].T) for d in range(2)])
        PRM = np.zeros((128, 3, 8), np.float32); BB = np.zeros((128, 2, 8, 16), np.float32); CC = np.zeros((128, 2, 8, 16), np.float32)
        for j in range(8):
            d, q = j // 4, j % 4
            for ch in range(2):
                g = gb * 8 + 2 * q + ch
                ps = slice(ch * 64, (ch + 1) * 64)
                PRM[ps, 0, j] = inp["s5_lambda_re"][i, d, g]; PRM[ps, 1, j] = inp["s5_lambda_im"][i, d, g]; PRM[ps, 2, j] = inp["s5_log_step"][i, d, g]
                BB[ps, 0, j] = inp["s5_b_re"][i, d, g]; BB[ps, 1, j] = inp["s5_b_im"][i, d, g]
                CC[ps, 0, j] = inp["s5_c_re"][i, d, g].T; CC[ps, 1, j] = inp["s5_c_im"][i, d, g].T
        maps.append({"U": U.astype(np.float32), "PRM": PRM, "BB": BB, "CC": CC, "IDENT": np.eye(128, dtype=np.float32)})
    res = run_bass_kernel_spmd(build_S5(), maps, core_ids=list(range(8)))
    o_lat = np.zeros((2, 2, 8192, 512), np.float32); o_ctx = np.zeros((2, 2, 256, 512), np.float32)
    for c in range(8):
        b, gb = c // 4, c % 4
        cols = slice(gb * 128, (gb + 1) * 128)
        Y = res.results[c]["Y"]
        for d in range(2):
            cc_, ll = s5_unseq(Y[d].T, d)
            o_lat[d, b][:, cols] = ll; o_ctx[d, b][:, cols] = cc_
    return o_lat, o_ctx


NCH = 66

def build_RET():
    kb = KB()
    QKV = kb.dram("QKV", [2, L_S5, 640], F32, "ExternalInput")
    DL = kb.dram("DL", [128, 2], F32, "ExternalInput")
    POS = kb.dram("POS", [128, 1], F32, "ExternalInput")
    MASK = kb.dram("MASK", [2, 128, 128], F32, "ExternalInput")
    IDENT = kb.dram("IDENT", [128, 128], F32, "ExternalInput")
    O = kb.dram("O", [2, L_S5, 128], F32, "ExternalOutput")
    idf, idb = make_ident(kb, IDENT)
    dl = kb.sb([128, 2], name="dl"); pos = kb.sb([128, 1], name="pos")
    msk = [kb.sb([128, 128], name=f"msk{d}") for d in range(2)]
    kb.dma("sp", dl[:], DL[:, :], writes=[dl]); kb.dma("sp", pos[:], POS[:, :], writes=[pos])
    for d in range(2):
        kb.dma("sp", msk[d][:], MASK[d, :, :], writes=[msk[d]])
    lg = kb.sb([128, 2], name="lg"); aq = kb.sb([128, 2], name="aq"); ak = kb.sb([128, 2], name="ak"); g128 = kb.sb([128, 2], name="g128")
    tq = kb.sb([128, 2], name="tq")
    kb.op("act", lambda e: e.activation(out=lg[:], in_=dl[:], func=AF.Exp, scale=-1.0), reads=[dl], writes=[lg])
    kb.op("dve", lambda e: e.tensor_scalar(out=lg[:], in0=lg[:], scalar1=1.0, scalar2=None, op0=ALU.add), reads=[lg], writes=[lg])
    kb.op("act", lambda e: e.activation(out=lg[:], in_=lg[:], func=AF.Ln), reads=[lg], writes=[lg])
    kb.op("dve", lambda e: e.tensor_scalar(out=lg[:], in0=lg[:], scalar1=-1.0, scalar2=None, op0=ALU.mult), reads=[lg], writes=[lg])
    kb.op("dve", lambda e: e.tensor_scalar(out=tq[:], in0=lg[:], scalar1=pos[:, 0:1], scalar2=None, op0=ALU.mult), reads=[lg, pos], writes=[tq])
    kb.op("act", lambda e: e.activation(out=aq[:], in_=tq[:], func=AF.Exp), reads=[tq], writes=[aq])
    kb.op("act", lambda e: e.activation(out=ak[:], in_=tq[:], func=AF.Exp, scale=-1.0), reads=[tq], writes=[ak])
    kb.op("dve", lambda e: e.tensor_scalar(out=ak[:], in0=ak[:], scalar1=128.0 ** -0.5, scalar2=None, op0=ALU.mult), reads=[ak], writes=[ak])
    kb.op("act", lambda e: e.activation(out=g128[:], in_=lg[:], func=AF.Exp, scale=128.0), reads=[lg], writes=[g128])
    xin = Ring([kb.sb([128, 640], name=f"xin{i}") for i in range(6)])
    rt = Ring([kb.sb([128, 4, 2, 64], name=f"rt{i}") for i in range(4)])
    ro = Ring([kb.sb([128, 2, 2, 64], name=f"ro{i}") for i in range(4)])
    qkb = Ring([kb.sb([128, 2, 128], BF16, name=f"qkb{i}") for i in range(4)])
    vb = Ring([kb.sb([128, 128], BF16, name=f"vb{i}") for i in range(4)])
    qkT = Ring([kb.sb([128, 2, 128], BF16, name=f"qkT{i}") for i in range(4)])
    sT = Ring([kb.sb([128, 128], BF16, name=f"sT{i}") for i in range(4)])
    ot = Ring([kb.sb([128, 128], name=f"ot{i}") for i in range(6)])
    Sfs = [kb.sb([128, 128], name=f"Sf{d}") for d in range(2)]; Sbs = [kb.sb([128, 128], BF16, name=f"Sb{d}") for d in range(2)]
    pT = Ring([kb.ps([128, 8, 128], BF16, name=f"pT{i}") for i in range(2)])
    psc = Ring([kb.ps([128, 512], name=f"psc{i}") for i in range(2)])
    pso = Ring([kb.ps([128, 512], name=f"pso{i}") for i in range(2)])
    pss = Ring([kb.ps([128, 512], name=f"pss{i}") for i in range(2)])
    for d in range(2):
        kb.op("dve", lambda e: e.memset(Sfs[d][:], 0.0), writes=[Sfs[d]])
        kb.op("dve", lambda e: e.memset(Sbs[d][:], 0.0), writes=[Sbs[d]])
    for c in range(NCH):
        for d in range(2):
            Sf, Sb = Sfs[d], Sbs[d]
            x = xin.next(); t_ = rt.next(); r_ = ro.next(); qk = qkb.next(); v_ = vb.next(); qT = qkT.next(); s_ = sT.next(); o = ot.next()
            kb.dma("sp", x[:], QKV[d, c * 128:(c + 1) * 128, :], writes=[x])
            qkv = x[:, 0:256].rearrange("p (w h e) -> p w h e", w=2, h=2)
            x1 = qkv[:, :, 0, :]; x2 = qkv[:, :, 1, :]
            cs2 = x[:, 384:512].rearrange("p (w e) -> p w e", w=2); sn2 = x[:, 512:640].rearrange("p (w e) -> p w e", w=2)
            kb.op("dve", lambda e: e.tensor_tensor(out=t_[:, 0, :, :], in0=x1, in1=cs2, op=ALU.mult), reads=[x], writes=[t_])
            kb.op("pool", lambda e: e.tensor_tensor(out=t_[:, 1, :, :], in0=x2, in1=sn2, op=ALU.mult), reads=[x], writes=[t_])
            kb.op("dve", lambda e: e.tensor_tensor(out=t_[:, 2, :, :], in0=x1, in1=sn2, op=ALU.mult), reads=[x], writes=[t_])
            kb.op("pool", lambda e: e.tensor_tensor(out=t_[:, 3, :, :], in0=x2, in1=cs2, op=ALU.mult), reads=[x], writes=[t_])
            kb.op("dve", lambda e: e.tensor_tensor(out=r_[:, :, 0, :], in0=t_[:, 0, :, :], in1=t_[:, 1, :, :], op=ALU.subtract), reads=[t_], writes=[r_])
            kb.op("dve", lambda e: e.tensor_tensor(out=r_[:, :, 1, :], in0=t_[:, 2, :, :], in1=t_[:, 3, :, :], op=ALU.add), reads=[t_], writes=[r_])
            kb.op("act", lambda e: e.activation(out=qk[:, 0, :], in_=r_[:, 0, :, :], func=AF.Copy, scale=aq[:, d:d + 1]), reads=[r_, aq], writes=[qk])
            kb.op("act", lambda e: e.activation(out=qk[:, 1, :], in_=r_[:, 1, :, :], func=AF.Copy, scale=ak[:, d:d + 1]), reads=[r_, ak], writes=[qk])
            kb.op("act", lambda e: e.activation(out=v_[:], in_=x[:, 256:384], func=AF.Copy), reads=[x], writes=[v_])
            p = pT.next()
            for w in range(2):
                kb.op("pe", lambda e: e.transpose(out=p[:, w, :], in_=qk[:, w, :], identity=idb[:]), reads=[qk, idb], writes=[p])
            kb.op("dve", lambda e: e.tensor_copy(out=qT[:], in_=p[:, 0:2, :]), reads=[p], writes=[qT])
            ps_ = psc.next()
            kb.op("pe", lambda e: e.matmul(out=ps_[:, 0:128], lhsT=qT[:, 1, :], rhs=qT[:, 0, :], start=True, stop=True), reads=[qT], writes=[ps_])
            kb.op("dve", lambda e: e.tensor_tensor(out=s_[:], in0=ps_[:, 0:128], in1=msk[d][:], op=ALU.mult), reads=[ps_, msk[d]], writes=[s_])
            po = pso.next()
            kb.op("pe", lambda e: e.matmul(out=po[:, 0:128], lhsT=s_[:], rhs=v_[:], start=True, stop=False), reads=[s_, v_], writes=[po])
            kb.op("pe", lambda e: e.matmul(out=po[:, 0:128], lhsT=qT[:, 0, :], rhs=Sb[:], start=False, stop=True), reads=[qT, Sb], writes=[po])
            kb.op("act", lambda e: e.activation(out=o[:], in_=po[:, 0:128], func=AF.Copy), reads=[po], writes=[o])
            kb.dma("act", O[d, c * 128:(c + 1) * 128, :], o[:], reads=[o], is_output=True)
            pS = pss.next()
            kb.op("pe", lambda e: e.matmul(out=pS[:, 0:128], lhsT=qk[:, 1, :], rhs=v_[:], start=True, stop=True), reads=[qk, v_], writes=[pS])
            kb.op("dve", lambda e: e.tensor_scalar(out=Sf[:], in0=Sf[:], scalar1=g128[:, d:d + 1], scalar2=None, op0=ALU.mult), reads=[Sf, g128], writes=[Sf])
            kb.op("dve", lambda e: e.scalar_tensor_tensor(out=Sf[:], in0=pS[:, 0:128], scalar=g128[:, d:d + 1], in1=Sf[:], op0=ALU.mult, op1=ALU.add), reads=[pS, Sf, g128], writes=[Sf])
            kb.op("act", lambda e: e.activation(out=Sb[:], in_=Sf[:], func=AF.Copy), reads=[Sf], writes=[Sb])
    return kb.finish()

def rope_tables():
    n_rows = 8192 // 64
    row = np.repeat(np.arange(n_rows, dtype=np.float32), 64)
    col = (np.arange(8192) % 64).astype(np.float32)
    inv = (10000.0 ** (-np.arange(32, dtype=np.float32) / 32)).astype(np.float32)
    ang = np.concatenate([row[:, None] * inv, col[:, None] * inv], axis=-1)
    return np.cos(ang).astype(np.float32), np.sin(ang).astype(np.float32)

def run_RET(p_lat, p_ctx, inp):
    cos, sin = rope_tables()
    cos_c = np.ones((256, 64), np.float32); sin_c = np.zeros((256, 64), np.float32)
    maps = []
    for c in range(8):
        b, h = c // 4, c % 4
        seqs = []
        for d in range(2):
            def sl(off):
                cols = slice(off + h * 128, off + (h + 1) * 128)
                return s5_seq(p_lat[:, :, cols], p_ctx[:, :, cols], b, d)
            cs = s5_seq(cos[None].repeat(2, 0), cos_c[None].repeat(2, 0), b, d)
            sn = s5_seq(sin[None].repeat(2, 0), sin_c[None].repeat(2, 0), b, d)
            seqs.append(np.concatenate([sl(512), sl(1024), sl(1536), cs, cs, sn, sn], axis=1))
        QKV = np.ascontiguousarray(np.stack(seqs)).astype(np.float32)
        DL = np.ascontiguousarray(np.broadcast_to(inp["ret_decay_logit"][0][:, h][None, :], (128, 2))).astype(np.float32)
        POS = (np.arange(128, dtype=np.float32) + 1)[:, None]
        jj, ii = np.meshgrid(np.arange(128), np.arange(128), indexing="ij")
        MASK = np.stack([(jj <= ii), (jj < ii)]).astype(np.float32)
        maps.append({"QKV": QKV, "DL": DL, "POS": POS, "MASK": MASK, "IDENT": np.eye(128, dtype=np.float32)})
    res = run_bass_kernel_spmd(build_RET(), maps, core_ids=list(range(8)))
    o_lat = np.zeros((2, 2, 8192, 512), np.float32); o_ctx = np.zeros((2, 2, 256, 512), np.float32)
    for c in range(8):
        b, h = c // 4, c % 4
        cols = slice(h * 128, (h + 1) * 128)
        Y = res.results[c]["O"]
        for d in range(2):
            cc_, ll = s5_unseq(Y[d], d)
            o_lat[d, b][:, cols] = ll; o_ctx[d, b][:, cols] = cc_
    return o_lat, o_ctx


def build_C0(NT=17, n_lat_tiles=16):
    D = 1024
    kb = KB()
    S5F = kb.dram("S5F", [NT * 128, 512], F32, "ExternalInput"); S5B = kb.dram("S5B", [NT * 128, 512], F32, "ExternalInput")
    RTF = kb.dram("RTF", [NT * 128, 512], F32, "ExternalInput"); RTB = kb.dram("RTB", [NT * 128, 512], F32, "ExternalInput")
    UU = kb.dram("UU", [NT * 128, 512], F32, "ExternalInput"); GG = kb.dram("GG", [NT * 128, 512], F32, "ExternalInput")
    X = kb.dram("X", [NT * 128, D], F32, "ExternalInput")
    WG = kb.dram("WG", [512, 1024], F32, "ExternalInput"); WO = kb.dram("WO", [1024, 1024], F32, "ExternalInput")
    REP = kb.dram("REP", [4, 128, 1024], F32, "ExternalInput")
    IDENT = kb.dram("IDENT", [128, 128], F32, "ExternalInput")
    O = kb.dram("O", [NT * 128, D], F32, "ExternalOutput")
    idf, idb = make_ident(kb, IDENT)
    stg = Ring([kb.sb([128, 1024], F32, name=f"stg{i}") for i in range(2)])
    wg = load_weight_bf16_q(kb, WG, 512, 1024, "wg", stg)
    wo = load_weight_bf16_q(kb, WO, 1024, 1024, "wo", stg)
    reps = []
    for i in range(4):
        r = kb.sb([128, 1024], name=f"rep{i}"); kb.dma("sp", r[:], REP[i, :, :], writes=[r]); reps.append(r)
    bglu, dgn, gate = reps[0], reps[1], (reps[2], reps[3])
    def ring(n, shape, dt=F32, nm="r"):
        return Ring([kb.sb(list(shape), dt, name=f"{nm}{i}") for i in range(n)])
    i_s5f = ring(2, [128, 512], nm="is5f"); i_s5b = ring(2, [128, 512], nm="is5b"); i_rf = ring(2, [128, 512], nm="irf"); i_rb = ring(2, [128, 512], nm="irb")
    i_u = ring(2, [128, 512], nm="iu"); i_g = ring(2, [128, 512], nm="ig"); i_x = ring(2, [128, D], nm="ix")
    y = kb.sb([128, 512], name="y"); w_ = kb.sb([128, 512], name="w_"); sg = kb.sb([128, 512], name="sg")
    ge = kb.sb([128, 512], BF16, name="ge"); geT = kb.sb([128, 4, 128], BF16, name="geT")
    a_ = kb.sb([128, 512], name="a_"); bg = kb.sb([128, 512], name="bg")
    cat = kb.sb([128, 1024], BF16, name="cat"); catT = kb.sb([128, 8, 128], BF16, name="catT")
    rr = kb.sb([128, 4, 128], name="rr"); xn = kb.sb([128, 4, 128], name="xn"); sq = kb.sb([128, 128], name="sq")
    sm = kb.sb([128, 4], name="sm"); s2 = kb.sb([128, 4], name="s2"); mn = kb.sb([128, 4], name="mn"); vr = kb.sb([128, 4], name="vr"); rsd = kb.sb([128, 4], name="rsd")
    slg = kb.sb([128, 512], name="slg")
    ot = ring(2, [128, D], nm="ot")
    pT = Ring([kb.ps([128, 8, 128], BF16, name=f"pT{i}") for i in range(2)])
    pm = Ring([kb.ps([128, 512], F32, name=f"pm{i}") for i in range(4)])
    for t in range(NT):
        rws = slice(t * 128, (t + 1) * 128)
        r = 0 if t < n_lat_tiles else 1
        s5f = i_s5f.next(); s5b = i_s5b.next(); rf = i_rf.next(); rb = i_rb.next(); u = i_u.next(); g = i_g.next(); x = i_x.next(); o = ot.next()
        for (buf, src) in ((s5f, S5F), (s5b, S5B), (rf, RTF), (rb, RTB), (u, UU), (g, GG), (x, X)):
            kb.dma("sp", buf[:], src[rws, :], writes=[buf])
        kb.op("dve", lambda e: e.tensor_tensor(out=y[:], in0=s5f[:], in1=s5b[:], op=ALU.add), reads=[s5f, s5b], writes=[y])
        kb.op("pool", lambda e: e.tensor_tensor(out=w_[:], in0=u[:], in1=dgn[:, 0:512], op=ALU.mult), reads=[u, dgn], writes=[w_])
        kb.op("dve", lambda e: e.tensor_tensor(out=y[:], in0=y[:], in1=w_[:], op=ALU.add), reads=[y, w_], writes=[y])
        kb.op("dve", lambda e: e.tensor_tensor(out=w_[:], in0=y[:], in1=y[:], op=ALU.mult), reads=[y], writes=[w_])
        kb.op("dve", lambda e: e.tensor_scalar(out=w_[:], in0=w_[:], scalar1=0.044715, scalar2=1.0, op0=ALU.mult, op1=ALU.add), reads=[w_], writes=[w_])
        kb.op("dve", lambda e: e.tensor_tensor(out=w_[:], in0=w_[:], in1=y[:], op=ALU.mult), reads=[w_, y], writes=[w_])
        kb.op("act", lambda e: e.activation(out=sg[:], in_=w_[:], func=AF.Sigmoid, scale=1.5957691216057308), reads=[w_], writes=[sg])
        kb.op("dve", lambda e: e.tensor_tensor(out=ge[:], in0=sg[:], in1=y[:], op=ALU.mult), reads=[sg, y], writes=[ge])
        transpose_tile(kb, ge, geT, idb, pT, 512)
        pa = pm.next(); pb = pm.next()
        for (pp, c0) in ((pa, 0), (pb, 512)):
            for k in range(4):
                kb.op("pe", lambda e: e.matmul(out=pp[:], lhsT=geT[:, k, :], rhs=wg[:, k, c0:c0 + 512], start=(k == 0), stop=(k == 3)), reads=[geT, wg], writes=[pp])
        kb.op("dve", lambda e: e.tensor_tensor(out=a_[:], in0=pa[:], in1=bglu[:, 0:512], op=ALU.add), reads=[pa, bglu], writes=[a_])
        kb.op("dve", lambda e: e.tensor_tensor(out=bg[:], in0=pb[:], in1=bglu[:, 512:1024], op=ALU.add), reads=[pb, bglu], writes=[bg])
        kb.op("act", lambda e: e.activation(out=bg[:], in_=bg[:], func=AF.Sigmoid), reads=[bg], writes=[bg])
        kb.op("dve", lambda e: e.tensor_tensor(out=cat[:, 0:512], in0=a_[:], in1=bg[:], op=ALU.mult), reads=[a_, bg], writes=[cat])
        rr2 = rr[:].rearrange("p h e -> p (h e)")
        kb.op("dve", lambda e: e.tensor_tensor(out=rr2, in0=rf[:], in1=rb[:], op=ALU.add), reads=[rf, rb], writes=[rr])
        kb.op("dve", lambda e: e.tensor_reduce(out=sm[:], in_=rr[:], axis=AX.X, op=ALU.add), reads=[rr], writes=[sm])
        for h in range(4):
            kb.op("act", lambda e: e.activation(out=sq[:], in_=rr[:, h, :], func=AF.Square, accum_out=s2[:, h:h + 1]), reads=[rr], writes=[sq, s2])
        kb.op("dve", lambda e: e.tensor_scalar(out=mn[:], in0=sm[:], scalar1=1.0 / 128, scalar2=None, op0=ALU.mult), reads=[sm], writes=[mn])
        kb.op("dve", lambda e: e.tensor_tensor(out=vr[:], in0=mn[:], in1=mn[:], op=ALU.mult), reads=[mn], writes=[vr])
        kb.op("dve", lambda e: e.scalar_tensor_tensor(out=vr[:], in0=s2[:], scalar=1.0 / 128, in1=vr[:], op0=ALU.mult, op1=ALU.subtract), reads=[s2, vr], writes=[vr])
        kb.op("dve", lambda e: e.tensor_scalar(out=vr[:], in0=vr[:], scalar1=EPS, scalar2=None, op0=ALU.add), reads=[vr], writes=[vr])
        kb.op("act", lambda e: e.activation(out=vr[:], in_=vr[:], func=AF.Sqrt), reads=[vr], writes=[vr])
        kb.op("dve", lambda e: e.reciprocal(out=rsd[:], in_=vr[:]), reads=[vr], writes=[rsd])
        for h in range(4):
            kb.op("dve", lambda e: e.tensor_scalar(out=xn[:, h, :], in0=rr[:, h, :], scalar1=mn[:, h:h + 1], scalar2=rsd[:, h:h + 1], op0=ALU.subtract, op1=ALU.mult),
                  reads=[rr, mn, rsd], writes=[xn])
        xn2 = xn[:].rearrange("p h e -> p (h e)")
        kb.op("act", lambda e: e.activation(out=slg[:], in_=g[:], func=AF.Silu), reads=[g], writes=[slg])
        kb.op("pool", lambda e: e.tensor_tensor(out=xn2, in0=xn2, in1=dgn[:, 512:1024], op=ALU.mult), reads=[xn, dgn], writes=[xn])
        kb.op("dve", lambda e: e.tensor_tensor(out=cat[:, 512:1024], in0=xn2, in1=slg[:], op=ALU.mult), reads=[xn, slg], writes=[cat])
        transpose_tile(kb, cat, catT, idb, pT, 1024)
        for cb in range(2):
            pp = pm.next(); cs = slice(cb * 512, (cb + 1) * 512)
            for k in range(8):
                kb.op("pe", lambda e: e.matmul(out=pp[:], lhsT=catT[:, k, :], rhs=wo[:, k, cs], start=(k == 0), stop=(k == 7)), reads=[catT, wo], writes=[pp])
            kb.op("dve", lambda e: e.tensor_tensor(out=o[:, cs], in0=pp[:], in1=gate[r][:, cs], op=ALU.mult), reads=[pp, gate[r]], writes=[o])
            kb.op("pool", lambda e: e.tensor_tensor(out=o[:, cs], in0=o[:, cs], in1=x[:, cs], op=ALU.add), reads=[o, x], writes=[o])
        kb.dma("act", O[rws, :], o[:], reads=[o], is_output=True)
    return kb.finish()

def run_C0(lat, ctx, s5f, s5b, rtf, rtb, p_lat, p_ctx, inp, m):
    tl = lambda a: to_tl(a[0], a[1])
    S5F, S5B, RTF, RTB = tl(s5f), tl(s5b), tl(rtf), tl(rtb)
    UU = to_tl(p_lat[:, :, 0:512], p_ctx[:, :, 0:512]); GG = to_tl(p_lat[:, :, 2048:2560], p_ctx[:, :, 2048:2560])
    X = to_tl(lat, ctx)
    maps = []
    for c in range(8):
        b = c // 4
        REP = np.stack([rep(inp["s5_b_glu"][0]), rep(np.concatenate([inp["s5_d"][0], inp["ret_gn_g"][0]])), rep(m[b, 0, 2]), rep(m[2, 0, 2])]).astype(np.float32)
        maps.append({"S5F": S5F[c], "S5B": S5B[c], "RTF": RTF[c], "RTB": RTB[c], "UU": UU[c], "GG": GG[c], "X": X[c],
                     "WG": inp["s5_w_glu"][0], "WO": inp["ev_w_out"][0], "REP": np.ascontiguousarray(REP), "IDENT": np.eye(128, dtype=np.float32)})
    res = run_bass_kernel_spmd(build_C0(), maps, core_ids=list(range(8)))
    return from_tl([r["O"] for r in res.results], 1024)


LP = 8456
LO = 8452

def build_QKV1():
    kb = KB()
    XM = kb.dram("XM", [4, 128, LP], F32, "ExternalInput")
    CW = kb.dram("CW", [128, 4, 6], F32, "ExternalInput")
    BD = kb.dram("BD", [3, 4, 128, 128], F32, "ExternalInput")
    OUT = kb.dram("OUT", [4, 4, 128, LO], F32, "ExternalOutput")
    cw = kb.sb([128, 4, 6], name="cw"); kb.dma("sp", cw[:], CW[:, :, :], writes=[cw])
    bdf = kb.sb([128, 12, 128], name="bdf"); bdb = kb.sb([128, 12, 128], BF16, name="bdb")
    for w in range(3):
        for t in range(4):
            kb.dma("sp", bdf[:, w * 4 + t, :], BD[w, t, :, :], writes=[bdf])
    kb.op("dve", lambda e: e.tensor_copy(out=bdb[:], in_=bdf[:]), reads=[bdf], writes=[bdb])
    xin = Ring([kb.sb([128, 516], name=f"xin{i}") for i in range(3)])
    acc = Ring([kb.sb([128, 512], name=f"acc{i}") for i in range(2)])
    xcf = Ring([kb.sb([128, 512], name=f"xcf{i}") for i in range(3)])
    xcb = Ring([kb.sb([128, 512], BF16, name=f"xcb{i}") for i in range(2)])
    xmb = Ring([kb.sb([128, 512], BF16, name=f"xmb{i}") for i in range(2)])
    oq = Ring([kb.sb([128, 3, 512], name=f"oq{i}") for i in range(2)])
    pm = Ring([kb.ps([128, 512], name=f"pm{i}") for i in range(6)])
    blocks = [(s0, min(512, LO - s0)) for s0 in range(0, LO, 512)]
    for t in range(4):
        for (s0, w) in blocks:
            x = xin.next(); a = acc.next(); xf = xcf.next(); xb = xcb.next(); mb = xmb.next(); o = oq.next()
            kb.dma("sp", x[:, 0:w + 4], XM[t, :, s0:s0 + w + 4], writes=[x])
            kb.op("dve", lambda e: e.tensor_scalar(out=a[:, 0:w], in0=x[:, 0:w], scalar1=cw[:, t, 0:1], scalar2=None, op0=ALU.mult), reads=[x, cw], writes=[a])
            for k in range(1, 5):
                kb.op("dve", lambda e: e.scalar_tensor_tensor(out=a[:, 0:w], in0=x[:, k:k + w], scalar=cw[:, t, k:k + 1], in1=a[:, 0:w], op0=ALU.mult, op1=ALU.add),
                      reads=[x, cw, a], writes=[a])
            kb.op("act", lambda e: e.activation(out=xf[:, 0:w], in_=a[:, 0:w], func=AF.Silu, bias=cw[:, t, 5:6]), reads=[a, cw], writes=[xf])
            kb.op("pool", lambda e: e.tensor_copy(out=xb[:, 0:w], in_=xf[:, 0:w]), reads=[xf], writes=[xb])
            kb.op("pool", lambda e: e.tensor_copy(out=mb[:, 0:w], in_=x[:, 2:2 + w]), reads=[x], writes=[mb])
            for wi in range(3):
                p = pm.next()
                src = mb if wi == 2 else xb
                kb.op("pe", lambda e: e.matmul(out=p[:, 0:w], lhsT=bdb[:, wi * 4 + t, :], rhs=src[:, 0:w], start=True, stop=True), reads=[bdb, src], writes=[p])
                kb.op("act", lambda e: e.activation(out=o[:, wi, 0:w], in_=p[:, 0:w], func=AF.Copy), reads=[p], writes=[o])
            for wi in range(3):
                kb.dma("act", OUT[wi, t, :, s0:s0 + w], o[:, wi, 0:w], reads=[o], is_output=True)
            kb.dma("act", OUT[3, t, :, s0:s0 + w], xf[:, 0:w], reads=[xf], is_output=True)
    return kb.finish()

def pad_nat(ctx_a, lat_a):
    C = lat_a.shape[1]
    z2 = np.zeros((2, C), np.float32)
    return np.concatenate([z2, ctx_a, z2, z2, lat_a, z2], 0)

def run_QKV1(p_lat, p_ctx, inp):
    maps = []
    for c in range(8):
        b, h = c // 4, c % 4
        cols = slice(h * 512, (h + 1) * 512)
        xp = pad_nat(p_ctx[b][:, cols], p_lat[b][:, cols])
        XM = np.ascontiguousarray(xp.T.reshape(4, 128, LP))
        CW = np.zeros((128, 4, 6), np.float32)
        cwf = inp["ml_conv_w"][0][:, cols]
        CW[:, :, 0:5] = cwf.T.reshape(4, 128, 5).transpose(1, 0, 2)
        CW[:, :, 5] = inp["ml_conv_b"][0][cols].reshape(4, 128).T
        BD = np.zeros((3, 4, 128, 128), np.float32)
        for wi, nm in enumerate(("ml_wq", "ml_wk", "ml_wv")):
            wblk = inp[nm][0][h * 128:(h + 1) * 128]
            for t in range(4):
                for nb in range(32):
                    BD[wi, t, 4 * nb:4 * nb + 4, 4 * nb:4 * nb + 4] = wblk[t * 32 + nb]
        maps.append({"XM": XM, "CW": CW, "BD": BD})
    res = run_bass_kernel_spmd(build_QKV1(), maps, core_ids=list(range(8)))
    outs = [(np.zeros((2, 8192, 2048), np.float32), np.zeros((2, 256, 2048), np.float32)) for _ in range(4)]
    for c in range(8):
        b, h = c // 4, c % 4
        cols = slice(h * 512, (h + 1) * 512)
        O = res.results[c]["OUT"]
        for wi in range(4):
            full = O[wi].reshape(512, LO).T
            outs[wi][1][b][:, cols] = full[0:256]
            outs[wi][0][b][:, cols] = full[260:260 + 8192]
    return outs

def build_ML():
    kb = KB()
    QKV = kb.dram("QKV", [2, L_S5, 1538], F32, "ExternalInput")
    GB = kb.dram("GB", [128, 4], F32, "ExternalInput")
    TRI = kb.dram("TRI", [2, 128, 128], F32, "ExternalInput")
    IDENT = kb.dram("IDENT", [128, 128], F32, "ExternalInput")
    O = kb.dram("O", [2, L_S5, 512], F32, "ExternalOutput")
    idf, idb = make_ident(kb, IDENT)
    gb = kb.sb([128, 4], name="gb"); kb.dma("sp", gb[:], GB[:, :], writes=[gb])
    tri = kb.sb([128, 128], name="tri"); ones = kb.sb([128, 128], name="ones")
    kb.dma("sp", tri[:], TRI[0, :, :], writes=[tri]); kb.dma("sp", ones[:], TRI[1, :, :], writes=[ones])
    onesb = kb.sb([128, 1], BF16, name="onesb")
    kb.op("dve", lambda e: e.tensor_copy(out=onesb[:], in_=ones[:, 0:1]), reads=[ones], writes=[onesb])
    gbn = kb.sb([128, 4], name="gbn"); gbi = kb.sb([128, 4], name="gbi")
    kb.op("dve", lambda e: e.tensor_scalar(out=gbn[:], in0=gb[:], scalar1=-1.0, scalar2=None, op0=ALU.mult), reads=[gb], writes=[gbn])
    kb.op("dve", lambda e: e.tensor_scalar(out=gbi[:], in0=gb[:], scalar1=float(np.log(512.0 ** -0.5)), scalar2=None, op0=ALU.add), reads=[gb], writes=[gbi])
    xin = Ring([kb.sb([128, 1538], name=f"xin{i}") for i in range(4)])
    sm = Ring([kb.sb([128, 8], name=f"sm{i}") for i in range(4)])
    qb = Ring([kb.sb([128, 512], BF16, name=f"qb{i}") for i in range(4)])
    kbf = Ring([kb.sb([128, 512], BF16, name=f"kbf{i}") for i in range(4)])
    vb = Ring([kb.sb([128, 512], BF16, name=f"vb{i}") for i in range(4)])
    qkT = Ring([kb.sb([128, 8, 128], BF16, name=f"qkT{i}") for i in range(4)])
    sT = Ring([kb.sb([128, 128], BF16, name=f"sT{i}") for i in range(4)])
    ot = Ring([kb.sb([128, 512], name=f"ot{i}") for i in range(4)])
    Cfs = [kb.sb([128, 4, 512], name=f"Cf{d}") for d in range(2)]; Cbs = [kb.sb([128, 4, 512], BF16, name=f"Cb{d}") for d in range(2)]
    nfs = [kb.sb([128, 4], name=f"nf{d}") for d in range(2)]; nbs = [kb.sb([128, 4], BF16, name=f"nb{d}") for d in range(2)]
    pT = Ring([kb.ps([128, 8, 128], BF16, name=f"pT{i}") for i in range(1)])
    psm = Ring([kb.ps([128, 512], name=f"psm{i}") for i in range(1)])
    psc = Ring([kb.ps([128, 512], name=f"psc{i}") for i in range(1)])
    pnum = Ring([kb.ps([128, 512], name=f"pnum{i}") for i in range(1)])
    pden = Ring([kb.ps([128, 512], name=f"pden{i}") for i in range(1)])
    pst = Ring([kb.ps([128, 512], name=f"pst{i}") for i in range(2)])
    pn = Ring([kb.ps([128, 512], name=f"pn{i}") for i in range(1)])
    for d in range(2):
        Cf, Cb, nf, nb_ = Cfs[d], Cbs[d], nfs[d], nbs[d]
        kb.op("dve", lambda e: e.memset(Cf[:], 0.0), writes=[Cf]); kb.op("pool", lambda e: e.memset(Cb[:], 0.0), writes=[Cb])
        kb.op("dve", lambda e: e.memset(nf[:], 0.0), writes=[nf]); kb.op("pool", lambda e: e.memset(nb_[:], 0.0), writes=[nb_])
    for c in range(NCH):
        for d in range(2):
            Cf, Cb, nf, nb_ = Cfs[d], Cbs[d], nfs[d], nbs[d]
            x = xin.next(); s = sm.next(); q_ = qb.next(); k_ = kbf.next(); v_ = vb.next(); T_ = qkT.next(); s_ = sT.next(); o = ot.next()
            kb.dma("sp", x[:], QKV[d, c * 128:(c + 1) * 128, :], writes=[x])
            kb.op("act", lambda e: e.activation(out=s[:, 0:1], in_=x[:, 1537:1538], func=AF.Exp, scale=-1.0, bias=gbn[:, 2 * d + 1:2 * d + 2]), reads=[x, gbn], writes=[s])
            kb.op("act", lambda e: e.activation(out=s[:, 0:1], in_=s[:, 0:1], func=AF.Ln, bias=ones[:, 0:1]), reads=[s, ones], writes=[s])
            kb.op("dve", lambda e: e.tensor_tensor(out=s[:, 1:2], in0=x[:, 1536:1537], in1=gbi[:, 2 * d:2 * d + 1], op=ALU.add), reads=[x, gbi], writes=[s])
            pc = psm.next()
            kb.op("pe", lambda e: e.matmul(out=pc[:, 0:2], lhsT=tri[:], rhs=s[:, 0:2], start=True, stop=True), reads=[tri, s], writes=[pc])
            kb.op("pe", lambda e: e.matmul(out=pc[:, 2:4], lhsT=ones[:], rhs=s[:, 0:2], start=True, stop=True), reads=[ones, s], writes=[pc])
            kb.op("act", lambda e: e.activation(out=s[:, 2:3], in_=pc[:, 0:1], func=AF.Exp, scale=-1.0), reads=[pc], writes=[s])
            kb.op("act", lambda e: e.activation(out=s[:, 3:4], in_=pc[:, 0:1], func=AF.Exp, bias=s[:, 1:2]), reads=[pc, s], writes=[s])
            kb.op("act", lambda e: e.activation(out=s[:, 4:5], in_=pc[:, 2:3], func=AF.Exp, scale=-1.0), reads=[pc], writes=[s])
            kb.op("act", lambda e: e.activation(out=q_[:], in_=x[:, 0:512], func=AF.Copy, scale=s[:, 2:3]), reads=[x, s], writes=[q_])
            kb.op("act", lambda e: e.activation(out=k_[:], in_=x[:, 512:1024], func=AF.Copy, scale=s[:, 3:4]), reads=[x, s], writes=[k_])
            kb.op("pool", lambda e: e.tensor_copy(out=v_[:], in_=x[:, 1024:1536]), reads=[x], writes=[v_])
            p = pT.next()
            for dt_ in range(4):
                kb.op("pe", lambda e: e.transpose(out=p[:, dt_, :], in_=q_[:, dt_ * 128:(dt_ + 1) * 128], identity=idb[:]), reads=[q_, idb], writes=[p])
            for dt_ in range(4):
                kb.op("pe", lambda e: e.transpose(out=p[:, 4 + dt_, :], in_=k_[:, dt_ * 128:(dt_ + 1) * 128], identity=idb[:]), reads=[k_, idb], writes=[p])
            kb.op("dve", lambda e: e.tensor_copy(out=T_[:], in_=p[:]), reads=[p], writes=[T_])
            ps_ = psc.next()
            for dt_ in range(4):
                kb.op("pe", lambda e: e.matmul(out=ps_[:, 0:128], lhsT=T_[:, 4 + dt_, :], rhs=T_[:, dt_, :], start=(dt_ == 0), stop=(dt_ == 3)), reads=[T_], writes=[ps_])
            kb.op("dve", lambda e: e.tensor_tensor(out=s_[:], in0=ps_[:, 0:128], in1=tri[:], op=ALU.mult), reads=[ps_, tri], writes=[s_])
            pn_ = pnum.next(); pd = pden.next()
            kb.op("pe", lambda e: e.matmul(out=pn_[:], lhsT=s_[:], rhs=v_[:], start=True, stop=False), reads=[s_, v_], writes=[pn_])
            for dt_ in range(4):
                kb.op("pe", lambda e: e.matmul(out=pn_[:], lhsT=T_[:, dt_, :], rhs=Cb[:, dt_, :], start=False, stop=(dt_ == 3)), reads=[T_, Cb], writes=[pn_])
            kb.op("pe", lambda e: e.matmul(out=pd[:, 0:1], lhsT=s_[:], rhs=onesb[:], start=True, stop=False), reads=[s_, onesb], writes=[pd])
            for dt_ in range(4):
                kb.op("pe", lambda e: e.matmul(out=pd[:, 0:1], lhsT=T_[:, dt_, :], rhs=nb_[:, dt_:dt_ + 1], start=False, stop=(dt_ == 3)), reads=[T_, nb_], writes=[pd])
            kb.op("act", lambda e: e.activation(out=s[:, 5:6], in_=pd[:, 0:1], func=AF.Abs), reads=[pd], writes=[s])
            kb.op("dve", lambda e: e.tensor_scalar(out=s[:, 5:6], in0=s[:, 5:6], scalar1=1.0, scalar2=None, op0=ALU.max), reads=[s], writes=[s])
            kb.op("dve", lambda e: e.reciprocal(out=s[:, 6:7], in_=s[:, 5:6]), reads=[s], writes=[s])
            kb.op("act", lambda e: e.activation(out=o[:], in_=pn_[:], func=AF.Copy, scale=s[:, 6:7]), reads=[pn_, s], writes=[o])
            kb.dma("act", O[d, c * 128:(c + 1) * 128, :], o[:], reads=[o], is_output=True)
            kb.op("dve", lambda e: e.tensor_scalar(out=Cf[:], in0=Cf[:], scalar1=s[:, 4:5], scalar2=None, op0=ALU.mult), reads=[Cf, s], writes=[Cf])
            pnn = pn.next()
            for dt_ in range(4):
                pS = pst.next()
                kb.op("pe", lambda e: e.matmul(out=pS[:], lhsT=k_[:, dt_ * 128:(dt_ + 1) * 128], rhs=v_[:], start=True, stop=True), reads=[k_, v_], writes=[pS])
                kb.op("dve", lambda e: e.scalar_tensor_tensor(out=Cf[:, dt_, :], in0=pS[:], scalar=s[:, 4:5], in1=Cf[:, dt_, :], op0=ALU.mult, op1=ALU.add), reads=[pS, s, Cf], writes=[Cf])
                kb.op("pe", lambda e: e.matmul(out=pnn[:, 2 * dt_:2 * dt_ + 1], lhsT=k_[:, dt_ * 128:(dt_ + 1) * 128], rhs=onesb[:], start=True, stop=True), reads=[k_, onesb], writes=[pnn])
            kb.op("act", lambda e: e.activation(out=Cb[:], in_=Cf[:], func=AF.Copy), reads=[Cf], writes=[Cb])
            kb.op("dve", lambda e: e.tensor_scalar(out=nf[:], in0=nf[:], scalar1=s[:, 4:5], scalar2=None, op0=ALU.mult), reads=[nf, s], writes=[nf])
            pnv = pnn[:, 0:8].rearrange("p (a b) -> p a b", b=2)[:, :, 0]
            kb.op("dve", lambda e: e.scalar_tensor_tensor(out=nf[:], in0=pnv, scalar=s[:, 4:5], in1=nf[:], op0=ALU.mult, op1=ALU.add), reads=[pnn, s, nf], writes=[nf])
            kb.op("dve", lambda e: e.tensor_copy(out=nb_[:], in_=nf[:]), reads=[nf], writes=[nb_])
    return kb.finish()

def run_ML(q, k, v, p_lat, p_ctx, inp):
    maps = []
    for c in range(8):
        b, h = c // 4, c % 4
        cols = slice(h * 512, (h + 1) * 512)
        seqs = []
        for d in range(2):
            parts = [s5_seq(a[0][:, :, cols], a[1][:, :, cols], b, d) for a in (q, k, v)]
            gi = 4096 + (2 * d) * 4 + h; gf = 4096 + (2 * d + 1) * 4 + h
            parts.append(s5_seq(p_lat[:, :, gi:gi + 1], p_ctx[:, :, gi:gi + 1], b, d))
            parts.append(s5_seq(p_lat[:, :, gf:gf + 1], p_ctx[:, :, gf:gf + 1], b, d))
            seqs.append(np.concatenate(parts, axis=1))
        QKV = np.ascontiguousarray(np.stack(seqs)).astype(np.float32)
        gbv = inp["ml_gate_b"][0].reshape(4, 4)[:, h]
        GB = np.ascontiguousarray(np.broadcast_to(gbv[None, :], (128, 4))).astype(np.float32)
        jj, ii = np.meshgrid(np.arange(128), np.arange(128), indexing="ij")
        TRI = np.stack([(jj <= ii), np.ones((128, 128), bool)]).astype(np.float32)
        maps.append({"QKV": QKV, "GB": GB, "TRI": TRI, "IDENT": np.eye(128, dtype=np.float32)})
    res = run_bass_kernel_spmd(build_ML(), maps, core_ids=list(range(8)))
    o_lat = np.zeros((2, 2, 8192, 2048), np.float32); o_ctx = np.zeros((2, 2, 256, 2048), np.float32)
    for c in range(8):
        b, h = c // 4, c % 4
        cols = slice(h * 512, (h + 1) * 512)
        Y = res.results[c]["O"]
        for d in range(2):
            cc_, ll = s5_unseq(Y[d], d)
            o_lat[d, b][:, cols] = ll; o_ctx[d, b][:, cols] = cc_
    return o_lat, o_ctx


def build_C1(NT=16):
    D = 1024; W = 2048
    kb = KB()
    HF = kb.dram("HF", [NT * 128, W], F32, "ExternalInput"); HB = kb.dram("HB", [NT * 128, W], F32, "ExternalInput")
    XC = kb.dram("XC", [NT * 128, W], F32, "ExternalInput"); OP = kb.dram("OP", [NT * 128, W], F32, "ExternalInput")
    X = kb.dram("X", [NT * 128, D], F32, "ExternalInput")
    WO = kb.dram("WO", [W, D], F32, "ExternalInput")
    REP = kb.dram("REP", [5, 128, 1024], F32, "ExternalInput")
    IDENT = kb.dram("IDENT", [128, 128], F32, "ExternalInput")
    O = kb.dram("O", [NT * 128, D], F32, "ExternalOutput")
    idf, idb = make_ident(kb, IDENT)
    stg = Ring([kb.sb([128, 1024], F32, name=f"stg{i}") for i in range(2)])
    wo = load_weight_bf16_q(kb, WO, W, D, "wo", stg)
    gn = kb.sb([128, W], name="gn"); sk = kb.sb([128, W], name="sk"); gate = kb.sb([128, D], name="gate")
    kb.dma("sp", gn[:, 0:1024], REP[0, :, :], writes=[gn]); kb.dma("sp", gn[:, 1024:2048], REP[1, :, :], writes=[gn])
    kb.dma("sp", sk[:, 0:1024], REP[2, :, :], writes=[sk]); kb.dma("sp", sk[:, 1024:2048], REP[3, :, :], writes=[sk])
    kb.dma("sp", gate[:], REP[4, :, :], writes=[gate])
    def ring(n, shape, dt=F32, nm="r"):
        return Ring([kb.sb(list(shape), dt, name=f"{nm}{i}") for i in range(n)])
    i_hf = ring(2, [128, W], nm="ihf"); i_hb = ring(2, [128, W], nm="ihb"); i_xc = ring(2, [128, W], nm="ixc"); i_op = ring(2, [128, W], nm="iop")
    i_x = ring(2, [128, D], nm="ix"); ot = ring(2, [128, D], nm="ot")
    rr = kb.sb([128, 4, 512], name="rr"); sq = kb.sb([128, 512], name="sq")
    zb = kb.sb([128, W], BF16, name="zb"); zT = kb.sb([128, 16, 128], BF16, name="zT")
    sm = kb.sb([128, 4], name="sm"); s2 = kb.sb([128, 4], name="s2"); mn = kb.sb([128, 4], name="mn"); vr = kb.sb([128, 4], name="vr"); rsd = kb.sb([128, 4], name="rsd")
    pT = Ring([kb.ps([128, 8, 128], BF16, name=f"pT{i}") for i in range(2)])
    pm = Ring([kb.ps([128, 512], F32, name=f"pm{i}") for i in range(4)])
    for t in range(NT):
        rws = slice(t * 128, (t + 1) * 128)
        hf = i_hf.next(); hb = i_hb.next(); xc = i_xc.next(); op_ = i_op.next(); x = i_x.next(); o = ot.next()
        for (buf, src) in ((hf, HF), (hb, HB), (xc, XC), (op_, OP), (x, X)):
            kb.dma("sp", buf[:], src[rws, :], writes=[buf])
        rr2 = rr[:].rearrange("p h e -> p (h e)")
        kb.op("dve", lambda e: e.tensor_tensor(out=rr2, in0=hf[:], in1=hb[:], op=ALU.add), reads=[hf, hb], writes=[rr])
        kb.op("dve", lambda e: e.tensor_reduce(out=sm[:], in_=rr[:], axis=AX.X, op=ALU.add), reads=[rr], writes=[sm])
        for h in range(4):
            kb.op("act", lambda e: e.activation(out=sq[:], in_=rr[:, h, :], func=AF.Square, accum_out=s2[:, h:h + 1]), reads=[rr], writes=[sq, s2])
        kb.op("dve", lambda e: e.tensor_scalar(out=mn[:], in0=sm[:], scalar1=1.0 / 512, scalar2=None, op0=ALU.mult), reads=[sm], writes=[mn])
        kb.op("dve", lambda e: e.tensor_tensor(out=vr[:], in0=mn[:], in1=mn[:], op=ALU.mult), reads=[mn], writes=[vr])
        kb.op("dve", lambda e: e.scalar_tensor_tensor(out=vr[:], in0=s2[:], scalar=1.0 / 512, in1=vr[:], op0=ALU.mult, op1=ALU.subtract), reads=[s2, vr], writes=[vr])
        kb.op("dve", lambda e: e.tensor_scalar(out=vr[:], in0=vr[:], scalar1=EPS, scalar2=None, op0=ALU.add), reads=[vr], writes=[vr])
        kb.op("act", lambda e: e.activation(out=vr[:], in_=vr[:], func=AF.Sqrt), reads=[vr], writes=[vr])
        kb.op("dve", lambda e: e.reciprocal(out=rsd[:], in_=vr[:]), reads=[vr], writes=[rsd])
        for h in range(4):
            kb.op("dve", lambda e: e.tensor_scalar(out=rr[:, h, :], in0=rr[:, h, :], scalar1=mn[:, h:h + 1], scalar2=rsd[:, h:h + 1], op0=ALU.subtract, op1=ALU.mult),
                  reads=[rr, mn, rsd], writes=[rr])
        kb.op("pool", lambda e: e.tensor_tensor(out=rr2, in0=rr2, in1=gn[:], op=ALU.mult), reads=[rr, gn], writes=[rr])
        kb.op("pool", lambda e: e.tensor_tensor(out=xc[:], in0=xc[:], in1=sk[:], op=ALU.mult), reads=[xc, sk], writes=[xc])
        kb.op("dve", lambda e: e.tensor_tensor(out=rr2, in0=rr2, in1=xc[:], op=ALU.add), reads=[rr, xc], writes=[rr])
        kb.op("act", lambda e: e.activation(out=op_[:], in_=op_[:], func=AF.Sigmoid), reads=[op_], writes=[op_])
        kb.op("dve", lambda e: e.tensor_tensor(out=zb[:], in0=rr2, in1=op_[:], op=ALU.mult), reads=[rr, op_], writes=[zb])
        transpose_tile(kb, zb, zT, idb, pT, W)
        for cb in range(2):
            pp = pm.next(); cs = slice(cb * 512, (cb + 1) * 512)
            for k in range(16):
                kb.op("pe", lambda e: e.matmul(out=pp[:], lhsT=zT[:, k, :], rhs=wo[:, k, cs], start=(k == 0), stop=(k == 15)), reads=[zT, wo], writes=[pp])
            kb.op("dve", lambda e: e.tensor_tensor(out=o[:, cs], in0=pp[:], in1=gate[:, cs], op=ALU.mult), reads=[pp, gate], writes=[o])
            kb.op("pool", lambda e: e.tensor_tensor(out=o[:, cs], in0=o[:, cs], in1=x[:, cs], op=ALU.add), reads=[o, x], writes=[o])
        kb.dma("act", O[rws, :], o[:], reads=[o], is_output=True)
    return kb.finish()

def run_C1(lat, hf_lat, hb_lat, xc_lat, p_lat, inp, m):
    HF = to_tl(hf_lat, None); HB = to_tl(hb_lat, None); XC = to_tl(xc_lat, None); OP = to_tl(p_lat[:, :, 2048:4096], None); X = to_tl(lat, None)
    g = inp["ml_gn_g"][0]; s = inp["ml_skip"][0]
    maps = []
    for c in range(8):
        b = c // 4
        REP = np.stack([rep(g[:1024]), rep(g[1024:]), rep(s[:1024]), rep(s[1024:]), rep(m[b, 1, 2])]).astype(np.float32)
        maps.append({"HF": HF[c], "HB": HB[c], "XC": XC[c], "OP": OP[c], "X": X[c], "WO": inp["ml_w_out"][0], "REP": np.ascontiguousarray(REP),
                     "IDENT": np.eye(128, dtype=np.float32)})
    res = run_bass_kernel_spmd(build_C1(), maps, core_ids=list(range(8)))
    lat2, _ = from_tl([r["O"] for r in res.results], 1024, has_ctx=False)
    return lat2


def build_M():
    kb = KB()
    cT = kb.dram("cT", [128, 8, 3], F32, "ExternalInput")
    W = kb.dram("W", [1024, 1536], F32, "ExternalInput")
    Bv = kb.dram("B", [3, 1536], F32, "ExternalInput")
    O = kb.dram("O", [3, 1536], F32, "ExternalOutput")
    cs = kb.sb([128, 8, 3]); sc = kb.sb([128, 8, 3]); bs = kb.sb([3, 1536]); os_ = kb.sb([3, 1536])
    ws = [kb.sb([128, 1536], name=f"w{k}") for k in range(8)]
    kb.dma("sp", cs[:], cT[:, :, :], writes=[cs])
    kb.dma("sp", bs[:], Bv[:, :], writes=[bs])
    for k in range(8):
        kb.dma("sp" if k % 2 == 0 else "act", ws[k][:], W[k * 128:(k + 1) * 128, :], writes=[ws[k]])
    kb.op("act", lambda e: e.activation(out=sc[:], in_=cs[:], func=AF.Silu), reads=[cs], writes=[sc])
    for j in range(3):
        pm = kb.ps([128, 512], name=f"pm{j}")
        for k in range(8):
            kb.op("pe", lambda e: e.matmul(out=pm[0:3, :], lhsT=sc[:, k, :], rhs=ws[k][:, j * 512:(j + 1) * 512],
                                          start=(k == 0), stop=(k == 7)), reads=[sc, ws[k]], writes=[pm])
        kb.op("dve", lambda e: e.tensor_tensor(out=os_[:, j * 512:(j + 1) * 512], in0=pm[0:3, :],
                                              in1=bs[:, j * 512:(j + 1) * 512], op=ALU.add), reads=[pm, bs], writes=[os_])
    kb.dma("sp", O[:, :], os_[:], reads=[os_], is_output=True)
    return kb.finish()

def run_M(inp):
    cv = np.stack([inp['c'][0], inp['c'][1], inp['c_ctx']], 0)
    cT = np.ascontiguousarray(cv.T.reshape(8, 128, 3).transpose(1, 0, 2))
    Wc = np.concatenate([inp['mod_w'][0], inp['mod_w'][1]], axis=1)
    bc = np.concatenate([inp['mod_b'][0], inp['mod_b'][1]], axis=0)
    maps = []
    for c in range(8):
        sl = slice(c * 1536, (c + 1) * 1536)
        maps.append({"cT": cT, "W": np.ascontiguousarray(Wc[:, sl]),
                     "B": np.ascontiguousarray(np.broadcast_to(bc[sl], (3, 1536)))})
    res = run_bass_kernel_spmd(build_M(), maps, core_ids=list(range(8)))
    m = np.concatenate([r["O"] for r in res.results], axis=1)
    return m.reshape(3, 2, 6, 1024)


def kernel(**inp):
    inp = {k: np.asarray(v) for k, v in inp.items()}
    x, ctx = inp["x"], inp["ctx"]
    m = run_M(inp)
    pl, pc = run_LIN(x, ctx, inp["ev_w_in"][0], inp["norm_mix_g"][0], m, 0, 0, 1)
    s5l, s5c = run_S5(pl[:, :, :512], pc[:, :, :512], inp)
    rl, rc = run_RET(pl, pc, inp)
    lat, cx = run_C0(x, ctx, (s5l[0], s5c[0]), (s5l[1], s5c[1]), (rl[0], rc[0]), (rl[1], rc[1]), pl, pc, inp, m)
    lat, cx = run_MLP(lat, cx, inp, m, 0, False)
    pl, pc = run_LIN(lat, cx, inp["ml_w_in"][0], inp["norm_mix_g"][1], m, 1, 0, 1)
    q, k, v, xc = run_QKV1(pl, pc, inp)
    hl, hc = run_ML(q, k, v, pl, pc, inp)
    lat = run_C1(lat, hl[0], hl[1], xc[0], pl, inp, m)
    out, _ = run_MLP(lat, None, inp, m, 1, True)
    return np.ascontiguousarray(out.astype(np.float32))
```
